# Optimizing a Trainium2 kernel written in Bass

```python
import math
import jax, jax.numpy as jnp
from jax import lax
import numpy as np

D_MODEL = 1024
BATCH = 4
SEQ = 4096
DEPTH = 1

CONV_DIM = 512
CONV_GROUPS = 8
CONV_K = 3
HGRN_HEADS = 4
HGRN_DK = 128
HGRN_DV = 128
HGRN_FDIM = HGRN_HEADS * HGRN_DK
HGRN_VDIM = HGRN_HEADS * HGRN_DV
CHUNK = 32
N_BRANCH = 2
IN_COL_SIZES = (CONV_DIM, CONV_DIM, CONV_DIM,
                HGRN_FDIM, HGRN_FDIM, HGRN_FDIM, HGRN_VDIM, HGRN_VDIM,
                D_MODEL, D_MODEL)
IN_COLS = 3 * CONV_DIM + 3 * HGRN_FDIM + 2 * HGRN_VDIM + N_BRANCH * D_MODEL
N_EXPERTS = 32
TOP_K = 4
D_FF = D_MODEL
SWIGLU_ALPHA = 1.702
SWIGLU_LIMIT = 7.0
MOE_BLOCK = 256
DN_ALPHA = (2.0 * DEPTH) ** 0.25
DN_BETA = (8.0 * DEPTH) ** -0.25
LN_EPS = 1e-5
RMS_EPS = 1e-6

kernel_name = "hybrid_conv_hgrn2_moe_deepnorm_encoder"


def layer_norm(x, g, b):
    xf = x.astype(jnp.float32)
    mu = jnp.mean(xf, axis=-1, keepdims=True)
    xc = xf - mu
    var = jnp.mean(xc * xc, axis=-1, keepdims=True)
    return (xc * lax.rsqrt(var + LN_EPS) * g.astype(jnp.float32) + b.astype(jnp.float32)).astype(x.dtype)


def split_cols(proj):
    outs, start = [], 0
    for size in IN_COL_SIZES:
        outs.append(proj[..., start:start + size])
        start += size
    return outs


def centred_short_conv(u, w):
    L = u.shape[1]
    half = CONV_K // 2
    up = jnp.pad(u, ((0, 0), (half, half), (0, 0)))
    return sum(w[j] * up[:, j:j + L] for j in range(CONV_K))


def hgrn2_chunked(q, k, v, logf):
    Bsz, L, H, dk = q.shape
    dv = v.shape[-1]
    n = L // CHUNK

    def to_chunks(t):
        return t.reshape(Bsz, n, CHUNK, H, t.shape[-1]).transpose(0, 3, 1, 2, 4)

    q, k, v, logf = to_chunks(q), to_chunks(k), to_chunks(v), to_chunks(logf)
    b = jnp.cumsum(logf, axis=3)
    b_ref = b[:, :, :, CHUNK // 2 - 1:CHUNK // 2]
    b_last = b[:, :, :, CHUNK - 1:CHUNK]
    scores = jnp.einsum('bhntd,bhnsd->bhnts', q * jnp.exp(b - b_ref), k * jnp.exp(b_ref - b))
    causal_in_dir = jnp.tril(jnp.ones((CHUNK, CHUNK), dtype=bool))
    scores = jnp.where(causal_in_dir, scores, 0.0)
    o_intra = jnp.einsum('bhnts,bhnse->bhnte', scores, v)
    kv = jnp.einsum('bhnsd,bhnse->bhnde', k * jnp.exp(b_last - b), v)
    decay = jnp.exp(b_last[:, :, :, 0])

    def step(S, inp):
        kv_c, dec_c = inp
        return dec_c[..., None] * S + kv_c, S

    S0 = jnp.zeros((Bsz, H, dk, dv), jnp.float32)
    _, S_prev = lax.scan(step, S0, (kv.transpose(2, 0, 1, 3, 4), decay.transpose(2, 0, 1, 3)))
    S_prev = S_prev.transpose(1, 2, 0, 3, 4)
    o_inter = jnp.einsum('bhntd,bhnde->bhnte', q * jnp.exp(b), S_prev)
    o = o_intra + o_inter
    return o.transpose(0, 2, 3, 1, 4).reshape(Bsz, L, H, dv)


def hgrn2_gate(f_raw, lb):
    f = lb + (1.0 - lb) * jax.nn.sigmoid(f_raw.astype(jnp.float32))
    return jnp.log(f), 1.0 - f


def token_mixer(u, w_in, conv_w, p_a, lb_fwd, lb_bwd, norm_w, p_b, w_o):
    Bsz, L, _ = u.shape
    proj = u @ w_in
    c_b, c_c, c_h, q_raw, f_fw, f_bw, i_raw, g_raw, gate_a, gate_b = split_cols(proj)
    y_a = c_b * centred_short_conv(c_c * c_h, conv_w)
    heads = lambda t: t.reshape(Bsz, L, HGRN_HEADS, -1)
    q = heads(jax.nn.silu(q_raw.astype(jnp.float32)))
    v = heads(i_raw.astype(jnp.float32))
    logf_f, k_f = hgrn2_gate(f_fw, lb_fwd)
    logf_b, k_b = hgrn2_gate(f_bw, lb_bwd)
    o_fwd = hgrn2_chunked(q, heads(k_f), v, heads(logf_f))
    flip = lambda t: jnp.flip(t, axis=1)
    o_bwd = flip(hgrn2_chunked(flip(q), flip(heads(k_b)), flip(v), flip(heads(logf_b))))
    o = o_fwd + o_bwd
    o = o * lax.rsqrt(jnp.mean(o * o, axis=-1, keepdims=True) + RMS_EPS)
    o = o * norm_w.astype(jnp.float32).reshape(HGRN_HEADS, HGRN_DV)
    y_b = (o.reshape(Bsz, L, HGRN_VDIM) * jax.nn.silu(g_raw.astype(jnp.float32))).astype(u.dtype)
    mix = jax.nn.sigmoid(gate_a) * (y_a @ p_a) + jax.nn.sigmoid(gate_b) * (y_b @ p_b)
    return mix @ w_o


def clamped_swiglu_expert(xblk, w1e, b1e, w2e, b2e):
    h = xblk @ w1e + b1e
    h_glu = jnp.minimum(h[:, :D_FF], SWIGLU_LIMIT)
    h_lin = jnp.clip(h[:, D_FF:], -SWIGLU_LIMIT, SWIGLU_LIMIT)
    a = h_glu * jax.nn.sigmoid(SWIGLU_ALPHA * h_glu) * (h_lin + 1.0)
    return a @ w2e + b2e


def moe_ffn(x, w_router, b_router, w1, b1, w2, b2):
    Bsz, L, D = x.shape
    xt = x.reshape(-1, D)
    T = xt.shape[0]
    logits = (xt @ w_router + b_router).astype(jnp.float32)
    top_vals, top_idx = lax.top_k(logits, TOP_K)
    gates = jax.nn.softmax(top_vals, axis=-1)
    N = T * TOP_K
    e_flat = top_idx.reshape(-1).astype(jnp.int32)
    tok_flat = jnp.arange(N, dtype=jnp.int32) // TOP_K
    g_flat = gates.reshape(-1)
    order = jnp.argsort(e_flat)
    e_sorted = e_flat[order]
    counts = jnp.bincount(e_flat, length=N_EXPERTS).astype(jnp.int32)
    padded = ((counts + MOE_BLOCK - 1) // MOE_BLOCK) * MOE_BLOCK
    start = jnp.cumsum(counts) - counts
    ends_p = jnp.cumsum(padded)
    pstart = ends_p - padded
    dest = pstart[e_sorted] + (jnp.arange(N, dtype=jnp.int32) - start[e_sorted])
    n_blocks = -(-N // MOE_BLOCK) + N_EXPERTS
    P = n_blocks * MOE_BLOCK
    buf_tok = jnp.zeros((P,), jnp.int32).at[dest].set(tok_flat[order])
    buf_gate = jnp.zeros((P,), jnp.float32).at[dest].set(g_flat[order])
    block_start = jnp.arange(n_blocks, dtype=jnp.int32) * MOE_BLOCK
    block_expert = jnp.clip(jnp.searchsorted(ends_p, block_start, side='right'), 0, N_EXPERTS - 1)
    xb = xt[buf_tok].reshape(n_blocks, MOE_BLOCK, D)

    def run_block(args):
        xblk, e = args
        return clamped_swiglu_expert(xblk, w1[e], b1[e], w2[e], b2[e])

    yb = lax.map(run_block, (xb, block_expert)).reshape(P, D)
    yb = yb * buf_gate[:, None].astype(yb.dtype)
    out = jax.ops.segment_sum(yb, buf_tok, num_segments=T)
    return out.reshape(Bsz, L, D).astype(x.dtype)


def setup_inputs(seed: int = 0) -> dict:
    key = jax.random.key(seed)
    ks = jax.random.split(key, 20)
    f32 = jnp.float32

    def nrm(k, shape, s):
        return jax.random.normal(k, shape, f32) * s

    return {
        "x": nrm(ks[0], (BATCH, SEQ, D_MODEL), 1.0),
        "w_in": nrm(ks[1], (DEPTH, D_MODEL, IN_COLS), D_MODEL ** -0.5),
        "conv_w": nrm(ks[2], (DEPTH, CONV_K, CONV_DIM), CONV_K ** -0.5),
        "p_a": nrm(ks[3], (DEPTH, CONV_DIM, D_MODEL), CONV_DIM ** -0.5),
        "hgrn_lb_logits": nrm(ks[4], (2, DEPTH + 1, HGRN_FDIM), 0.3),
        "hgrn_norm_w": 1.0 + nrm(ks[5], (DEPTH, HGRN_VDIM), 0.02),
        "p_b": nrm(ks[6], (DEPTH, HGRN_VDIM, D_MODEL), HGRN_VDIM ** -0.5),
        "w_o": nrm(ks[7], (DEPTH, D_MODEL, D_MODEL), D_MODEL ** -0.5 * DN_BETA),
        "ln1_g": 1.0 + nrm(ks[8], (DEPTH, D_MODEL), 0.02),
        "ln1_b": nrm(ks[9], (DEPTH, D_MODEL), 0.02),
        "w_router": nrm(ks[10], (DEPTH, D_MODEL, N_EXPERTS), D_MODEL ** -0.5),
        "b_router": nrm(ks[11], (DEPTH, N_EXPERTS), 0.01),
        "w1": nrm(ks[12], (DEPTH, N_EXPERTS, D_MODEL, 2 * D_FF), D_MODEL ** -0.5),
        "b1": nrm(ks[13], (DEPTH, N_EXPERTS, 2 * D_FF), 0.01),
        "w2": nrm(ks[14], (DEPTH, N_EXPERTS, D_FF, D_MODEL), D_FF ** -0.5 * DN_BETA),
        "b2": nrm(ks[15], (DEPTH, N_EXPERTS, D_MODEL), 0.01),
        "ln2_g": 1.0 + nrm(ks[16], (DEPTH, D_MODEL), 0.02),
        "ln2_b": nrm(ks[17], (DEPTH, D_MODEL), 0.02),
    }


def reference(x, w_in, conv_w, p_a, hgrn_lb_logits, hgrn_norm_w, p_b, w_o, ln1_g, ln1_b,
              w_router, b_router, w1, b1, w2, b2, ln2_g, ln2_b):
    lb_table = jnp.cumsum(jax.nn.softmax(hgrn_lb_logits.astype(jnp.float32), axis=1), axis=1)
    h = x
    for l in range(DEPTH):
        mixed = token_mixer(h, w_in[l], conv_w[l], p_a[l], lb_table[0, l], lb_table[1, l],
                            hgrn_norm_w[l], p_b[l], w_o[l])
        h = layer_norm(DN_ALPHA * h + mixed, ln1_g[l], ln1_b[l])
        ffn = moe_ffn(h, w_router[l], b_router[l], w1[l], b1[l], w2[l], b2[l])
        h = layer_norm(DN_ALPHA * h + ffn, ln2_g[l], ln2_b[l])
    return h
```

```python
import numpy as np
from contextlib import ExitStack
import concourse.bass as bass
import concourse.mybir as mybir
from concourse.bass_utils import run_bass_kernel_spmd

F32 = mybir.dt.float32
BF16 = mybir.dt.bfloat16
AF = mybir.ActivationFunctionType
ALU = mybir.AluOpType

D = 1024
SEQ = 4096
HALF = 2048
NT = 16
NE = 32
DFF = 1024
ALPHA = 2.0 ** 0.25
LN_EPS = 1e-5
RMS_EPS = 1e-6
NDMA = 24
DBG = {}
FULLS = ("full", "t_router", "m_router", "m_full") + tuple("%s_e%d" % (a, n) for a in "tm" for n in (1, 2, 4, 8, 16))

C_B, C_C, C_H, C_Q, C_FF, C_FB, C_I, C_G, C_GA, C_GB = 0, 512, 1024, 1536, 2048, 2560, 3072, 3584, 4096, 5120

K_MKF, K_MKB, K_M3F, K_M3B, K_ID, K_ONE = 0, 260, 520, 648, 776, 904
K_EC, K_LT = 1032, 1064
NCONST = 1192
CAP = 384


def make_consts():
    c = np.zeros((128, NCONST), np.float32)
    s = np.arange(128)[:, None]
    t = np.arange(128)[None, :]
    same = (s // 32) == (t // 32)
    cum_f = same & (s <= t)
    cum_b = same & (s >= t)
    ref_f = same & ((s % 32) <= 15)
    ref_b = same & ((s % 32) >= 16)
    c[:, K_MKF:K_MKF + 128] = cum_f.astype(np.float32) - ref_f.astype(np.float32)
    c[:, K_MKF + 128:K_MKF + 256] = cum_f
    c[:, K_MKB:K_MKB + 128] = cum_b.astype(np.float32) - ref_b.astype(np.float32)
    c[:, K_MKB + 128:K_MKB + 256] = cum_b
    for k in range(4):
        c[:, K_MKF + 256 + k] = (np.arange(128) // 32 == k)
        c[:, K_MKB + 256 + k] = (np.arange(128) // 32 == 3 - k)
    c[:, K_M3F:K_M3F + 128] = same & (s > t)
    c[:, K_M3B:K_M3B + 128] = same & (s < t)
    c[:, K_ID:K_ID + 128] = np.eye(128)
    c[:, K_ONE:K_ONE + 128] = 1.0
    c[:, K_EC:K_EC + NE] = (np.arange(NE) * CAP)[None, :]
    c[:, K_LT:K_LT + 128] = (s < t)
    return c


def ne_decl(stage):
    if "_e" in stage:
        return int(stage.split("_e")[1])
    return 0 if "router" in stage else NE


class Buf:
    __slots__ = ("w", "r", "excl")

    def __init__(self, excl=False):
        self.w = None
        self.r = {}
        self.excl = excl


class KB:
    def __init__(self, nc):
        self.nc = nc
        self.E = dict(pe=nc.tensor, act=nc.scalar, dve=nc.vector, pool=nc.gpsimd, sp=nc.sync)
        self.sem = {k: nc.alloc_semaphore("s_" + k) for k in self.E}
        self.cnt = {k: 0 for k in self.E}
        self.seen = {k: {} for k in self.E}
        self.dsem = [[nc.alloc_semaphore("d%d" % i), 0] for i in range(2 * NDMA)]
        self.dnext = {"sp": 0, "pool": 0}
        self.nwait = 0

    def _deps(self, reads, writes, e=None):
        deps = {}

        def add(k, v):
            if deps.get(k, 0) < v:
                deps[k] = v
        for b in reads:
            if b.w is not None:
                add(*b.w)
            if b.excl:
                for k, v in b.r.items():
                    if k != e:
                        add(k, v)
        for b in writes:
            if b.w is not None:
                add(*b.w)
            for k, v in b.r.items():
                add(k, v)
        return deps

    def _wait(self, e, deps):
        for k, v in deps.items():
            if k == e and e in ("pe", "sp"):
                continue
            if self.seen[e].get(k, 0) >= v:
                continue
            self.seen[e][k] = v
            sem = self.sem[k] if isinstance(k, str) else self.dsem[k][0]
            self.E[e].wait_ge(sem, v)
            self.nwait += 1

    def _mark(self, ev, reads, writes):
        k, v = ev
        for b in reads:
            if b.r.get(k, 0) < v:
                b.r[k] = v
        for b in writes:
            b.w = ev
            b.r = {}

    def op(self, e, fn, reads=(), writes=(), signal=True):
        self._wait(e, self._deps(reads, writes, e))
        ins = fn(self.E[e])
        if signal:
            self.cnt[e] += 1
            ins.then_inc(self.sem[e], 1)
            ev = (e, self.cnt[e])
        else:
            ev = (e, self.cnt[e] + 1)
        self._mark(ev, reads, writes)
        return ev

    def dma(self, q, out, in_, reads=(), writes=(), **kw):
        deps = self._deps(reads, writes)
        idx = self.dnext[q] + (NDMA if q == "pool" else 0)
        self.dnext[q] = (self.dnext[q] + 1) % NDMA
        sem, val = self.dsem[idx]
        if val > 0 and deps.get(idx, 0) < val:
            deps[idx] = val
        self._wait(q, deps)
        ins = self.E[q].dma_start(out=out, in_=in_, **kw)
        val += 16
        self.dsem[idx][1] = val
        ins.then_inc(sem, 16)
        ev = (idx, val)
        self._mark(ev, reads, writes)
        return ev

    def idma(self, out, in_, idx_ap, scatter, bound, reads=(), writes=()):
        q = "pool"
        deps = self._deps(reads, writes)
        idx = self.dnext[q] + NDMA
        self.dnext[q] = (self.dnext[q] + 1) % NDMA
        sem, val = self.dsem[idx]
        if val > 0 and deps.get(idx, 0) < val:
            deps[idx] = val
        self._wait(q, deps)
        off = bass.IndirectOffsetOnAxis(ap=idx_ap, axis=0)
        if not hasattr(self, "_bound_regs"):
            self._bound_regs = {}
        if bound not in self._bound_regs:
            self._bound_regs[bound] = self.E[q].to_reg(bound)
        bound = self._bound_regs[bound]
        if scatter:
            ins = self.E[q].indirect_dma_start(out=out, out_offset=off, in_=in_, in_offset=None, bounds_check=bound, oob_is_err=False)
        else:
            ins = self.E[q].indirect_dma_start(out=out, out_offset=None, in_=in_, in_offset=off, bounds_check=bound, oob_is_err=False)
        val += 16
        self.dsem[idx][1] = val
        ins.then_inc(sem, 16)
        ev = (idx, val)
        self._mark(ev, reads, writes)
        return ev

    def barrier(self):
        for e in self.E:
            deps = {k: v for k, v in self.cnt.items() if v > 0}
            for i, (sem, val) in enumerate(self.dsem):
                if val > 0:
                    deps[i] = val
            self._wait(e, deps)

    def wait_ev(self, e, ev):
        self._wait(e, {ev[0]: ev[1]})


class Ctx:
    pass


def build_nc(stage="full"):
    nc = bass.Bass("TRN2", target_bir_lowering=False)
    kb = KB(nc)
    g = Ctx()
    g.nc, g.kb = nc, kb

    def din(name, shape):
        return nc.dram_tensor(name, list(shape), F32, kind="ExternalInput").ap()

    g.xT = din("xT", [D, SEQ])
    g.xo = din("xo", [HALF, D])
    g.w_in = din("w_in", [D, 6144])
    g.convw = din("convw_t", [512, 3])
    g.lbl = din("lbl", [1, 2048])
    g.lblt = din("lbl_t", [512, 4])
    g.normw = din("normw_t", [128, 4])
    g.p_a = din("p_a", [512, D])
    g.p_b = din("p_b", [512, D])
    g.w_o = din("w_o", [D, D])
    g.ln1g = din("ln1_g", [1, D])
    g.ln1b = din("ln1_b", [1, D])
    g.consts = din("consts", [128, NCONST])
    if stage in FULLS:
        g.w_r = din("w_router", [D, NE])
        g.b_r = din("b_router", [1, NE])
        if "router" not in stage:
            nd = ne_decl(stage)
            g.w1 = din("w1", [nd, D, 2 * DFF])
            g.b1 = din("b1t", [nd, 128, 16])
            g.w2 = din("w2", [nd, DFF, D])
            g.b2 = din("b2", [nd, D])
        g.ln2g = din("ln2_g", [1, D])
        g.ln2b = din("ln2_b", [1, D])
    g.out = nc.dram_tensor("out", [HALF, D], F32, kind="ExternalOutput").ap()
    if stage.startswith("m_"):
        g.hs = din("h_in", [HALF, D])
    else:
        g.hs = nc.dram_tensor("h_scratch", [HALF, D], F32, kind="Internal").ap()

    g.banks = [(nc.alloc_psum_tensor("bank%d" % i, [128, 512], F32), Buf(excl=True)) for i in range(8)]
    g.bnext = 0

    def bank():
        b = g.banks[g.bnext]
        g.bnext = (g.bnext + 1) % 8
        return b
    g.bank = bank
    g.pools = {"P": [0, 1], "E": [2, 3, 4, 5], "K": [6, 7]}
    g.pnext = {"P": 0, "E": 0, "K": 0}

    def bank_from(name):
        lst = g.pools[name]
        b = g.banks[lst[g.pnext[name] % len(lst)]]
        g.pnext[name] += 1
        return b
    g.bank_from = bank_from

    g.cst = nc.alloc_sbuf_tensor("cst", [128, NCONST], F32)
    g.cstb = Buf()
    kb.dma("sp", g.cst[:], g.consts[:, :], writes=[g.cstb])
    g.idb = nc.alloc_sbuf_tensor("idb", [128, 128], BF16)
    g.idbb = Buf()
    kb.op("dve", lambda e: e.tensor_copy(out=g.idb[:], in_=g.cst[:, K_ID:K_ID + 128]), reads=[g.cstb], writes=[g.idbb])

    if not stage.startswith("m_"):
        with ExitStack() as es0:
            mixer(g, es0, stage)
    if stage == "t_hs":
        with ExitStack() as es:
            al = allocator(nc, es)
            rr = Ring(al, "thsr", [128, D], F32, 2)
            g.out_evs = []
            for T in range(NT):
                t_, b_ = rr.get()
                kb.dma("sp", t_[:], g.hs[T * 128:(T + 1) * 128, :], writes=[b_])
                g.out_evs.append(kb.dma("sp", g.out[T * 128:(T + 1) * 128, :], t_[:], reads=[b_]))
    elif not stage.startswith("mixer"):
        moe(g, stage)
    for ev in g.out_evs + getattr(g, "dbg_evs", []) + getattr(g, "dbg_evs2", []):
        kb.wait_ev("sp", ev)
    return nc


class Ring:
    def __init__(self, al, name, shape, dtype, n):
        self.t = [al("%s%d" % (name, i), shape, dtype) for i in range(n)]
        self.b = [Buf() for _ in range(n)]
        self.i = 0
        self.n = n

    def get(self):
        r = (self.t[self.i], self.b[self.i])
        self.i = (self.i + 1) % self.n
        return r


def mm_group(g, out_ap, outbuf, pairs, reads, f32=False, start=True, stop=True):
    kb = g.kb
    n = len(pairs)
    ev = None
    for i, (l, r) in enumerate(pairs):
        st = start and i == 0
        sp = stop and i == n - 1
        ev = kb.op("pe", lambda e, l=l, r=r, st=st, sp=sp: e.matmul(out_ap, lhsT=l, rhs=r, start=st, stop=sp),
                   reads=reads, writes=[outbuf], signal=(i == n - 1))
    return ev


def allocator(nc, es):
    def al(name, shape, dtype):
        return es.enter_context(nc.sbuf_tensor(name, list(shape), dtype))
    return al


def xT_dma(g, dst, t0, n, buf):
    return g.kb.dma("pool", dst, g.xT[:, t0:t0 + n].rearrange("(k p) t -> p k t", p=128), writes=[buf])


def w_dma(g, dst, src, buf):
    return g.kb.dma("pool", dst, src.rearrange("(k p) n -> p k n", p=128), writes=[buf])


def mixer(g, es0, stage):
    nc, kb = g.nc, g.kb
    cst, cstb = g.cst, g.cstb
    op = kb.op
    al0 = allocator(nc, es0)

    xTo = al0("xTo", [128, 8, HALF + 8], BF16)
    xTob = [Buf() for _ in range(5)]
    for B in range(4):
        xT_dma(g, xTo[:, :, B * 512:(B + 1) * 512], B * 512, 512, xTob[B])
    xT_dma(g, xTo[:, :, HALF:HALF + 8], HALF, 8, xTob[4])
    ybT = al0("ybT", [128, 4, HALF], BF16)
    ybb = [[Buf() for _ in range(4)] for _ in range(4)]
    yaT = al0("yaT", [128, 4, HALF], BF16)
    yab = [Buf() for _ in range(4)]

    mk = [cst[:, K_MKF:K_MKF + 260], cst[:, K_MKB:K_MKB + 260]]
    m3 = [cst[:, K_M3F:K_M3F + 128], cst[:, K_M3B:K_M3B + 128]]
    ones = cst[:, K_ONE:K_ONE + 128]

    def cmask(dr, k):
        base = K_MKF if dr == 0 else K_MKB
        return cst[:, base + 256 + k:base + 257 + k]

    with ExitStack() as es:
        al = allocator(nc, es)
        lraw = al("lraw", [128, 4, 4], F32)
        lrawb = Buf()
        kb.dma("sp", lraw[:], g.lblt.rearrange("(h p) c -> p h c", p=128), writes=[lrawb])
        omlc = al("omlc", [128, 2, 4], F32)
        omlcb = Buf()
        for a in range(2):
            op("dve", lambda e, a=a: e.tensor_tensor(out=omlc[:, a, :], in0=lraw[:, :, 2 * a + 1], in1=lraw[:, :, 2 * a],
                                                     op=ALU.subtract), reads=[lrawb], writes=[omlcb])
        op("act", lambda e: e.activation(out=omlc[:], in_=omlc[:], func=AF.Sigmoid), reads=[omlcb], writes=[omlcb])
        omlt = al("omlt", [128, 2, 512], F32)
        omltb = Buf()
        with ExitStack() as es2:
            lbig = allocator(nc, es2)("lbig", [128, 2048], F32)
            lbigb = Buf()
            kb.dma("sp", lbig[:], g.lbl[0:1, :].broadcast_to([128, 2048]), writes=[lbigb])
            for a in range(2):
                op("dve", lambda e, a=a: e.tensor_tensor(out=omlt[:, a, :], in0=lbig[:, a * 1024 + 512:a * 1024 + 1024],
                                                         in1=lbig[:, a * 1024:a * 1024 + 512], op=ALU.subtract),
                   reads=[lbigb], writes=[omltb])
            kb.barrier()
        op("act", lambda e: e.activation(out=omlt[:], in_=omlt[:], func=AF.Sigmoid), reads=[omltb], writes=[omltb])
        nrm = al("nrm", [128, 4], F32)
        nrmb = Buf()
        kb.dma("sp", nrm[:], g.normw[:, :], writes=[nrmb])

        w_tm = al("w_tm", [128, 8, 384], BF16)
        w_cm = al("w_cm", [128, 8, 512], BF16)
        wtmb, wcmb = Buf(), Buf()
        names = ["qt", "Qh", "kt"]
        big = {(n, dr): al("%s%d" % (n, dr), [128, HALF], BF16) for n in names for dr in range(2)}
        bigb = {(n, dr): [Buf() for _ in range(NT)] for n in names for dr in range(2)}
        V = al("V", [128, NT, 128], BF16)
        Vb = [Buf() for _ in range(NT)]
        sgl = al("sgl", [128, HALF], BF16)
        sglb = [Buf() for _ in range(4)]
        kv = [al("kv%d" % i, [128, 128, 64], BF16) for i in range(2)]
        SCM = 8
        decr = al("decr", [128, SCM, 64], F32)
        decrb = Buf()
        kvb = [Buf() for _ in range(2)]
        dec = [al("dec%d" % i, [128, 64], F32) for i in range(2)]
        decb = [Buf() for _ in range(2)]
        bound = al("bound", [128, 128], F32)
        boundb = Buf()
        bnd16 = al("bnd16", [128, 128], BF16)
        bnd16b = Buf()
        xoth = Ring(al, "xoth", [128, 8, 512], BF16, 2)

        r_sg = Ring(al, "r_sg", [128, 256], F32, 2)
        r_omf = Ring(al, "r_omf", [128, 256], F32, 5)
        r_lf = Ring(al, "r_lf", [128, 256], F32, 2)
        r_ex3 = Ring(al, "r_ex3", [128, 256], F32, 2)
        r_ex12 = Ring(al, "r_ex12", [128, 2, 256], F32, 2)
        r_exm = Ring(al, "r_exm", [128, 2, 128], F32, 2)
        r_khm = Ring(al, "r_khm", [128, 2, 4, 128], BF16, 2)
        r_vo = Ring(al, "r_vo", [128, 128], BF16, 7)
        r_qT = Ring(al, "r_qT", [128, 512], BF16, 3)
        r_sgT = Ring(al, "r_sgT", [128, 2, 512], BF16, 3)
        r_scm = Ring(al, "r_scm", [128, 256], BF16, 2)
        r_f512 = Ring(al, "r_f512", [128, 512], F32, 2)

        def run_pipeline(items, stages):
            ns = len(stages)
            cs = []
            for it in items:
                c = Ctx()
                c.T = it
                cs.append(c)
            if DBG.get("noskew", 0):
                for c in cs:
                    for st_ in stages:
                        st_(c)
                return
            for step in range(len(cs) + ns - 1):
                for si in range(ns):
                    i = step - si
                    if 0 <= i < len(cs):
                        stages[si](cs[i])

        def scan_all(x, fold):
            if fold:
                op("dve", lambda e: e.scalar_tensor_tensor(out=kv[x][:, :, 0], in0=bound[:], scalar=dec[x][:, 0:1], in1=kv[x][:, :, 0],
                                                           op0=ALU.mult, op1=ALU.add), reads=[boundb, decb[x], kvb[x]], writes=[kvb[x]])
            op("act", lambda e: e.activation(out=decr[:], in_=dec[x][:].unsqueeze(1).broadcast_to([128, SCM, 64]), func=AF.Copy),
               reads=[decb[x]], writes=[decrb])
            op("pool", lambda e: e.memset(decr[:, :, 0:1], 0.0), reads=[decrb], writes=[decrb])
            for eg in range(128 // SCM):
                view = kv[x][:, eg * SCM:(eg + 1) * SCM, :].rearrange("p e c -> p (e c)")
                op("dve", lambda e, view=view: e.tensor_tensor_scan(out=view, data0=decr[:].rearrange("p m c -> p (m c)"), data1=view,
                                                                    initial=0.0, op0=ALU.mult, op1=ALU.add),
                   reads=[decrb], writes=[kvb[x]])

        def load_head(hh):
            for i, c0 in enumerate([C_FF, C_FB, C_I]):
                w_dma(g, w_tm[:, :, i * 128:(i + 1) * 128], g.w_in[:, c0 + hh * 128:c0 + (hh + 1) * 128], wtmb)
            for i, c0 in enumerate([C_Q, C_FF, C_FB, C_G]):
                w_dma(g, w_cm[:, :, i * 128:(i + 1) * 128], g.w_in[:, c0 + hh * 128:c0 + (hh + 1) * 128], wcmb)
            pre = xoth.get()
            xT_dma(g, pre[0][:], HALF, 512, pre[1])
            return pre

        xpre = load_head(0)
        for h in range(4):
            hs = slice(h * 128, (h + 1) * 128)

            oth = {}

            def a0(c):
                T = c.T
                Bo, tt = (T - 16) // 4, (T - 16) % 4
                if tt == 0:
                    if Bo == 0:
                        oth[Bo] = xpre
                    else:
                        oth[Bo] = xoth.get()
                        xT_dma(g, oth[Bo][0][:], HALF + Bo * 512, 512, oth[Bo][1])
                xt, xb = oth[Bo]
                c.c0 = 124 - 4 * T
                c.P, c.Pb = g.bank_from("P")
                mm_group(g, c.P[:, 0:256], c.Pb, [(xt[:, k, tt * 128:(tt + 1) * 128], w_tm[:, k, 128:384]) for k in range(8)],
                         reads=[xb, wtmb])

            def a1(c):
                c.sg, c.sgb = r_sg.get()
                op("act", lambda e: e.activation(out=c.sg[:, 0:128], in_=c.P[:, 0:128], func=AF.Exp), reads=[c.Pb], writes=[c.sgb])
                op("act", lambda e: e.activation(out=c.sg[:, 0:128], in_=c.sg[:, 0:128], func=AF.Ln, bias=1.0), reads=[c.sgb], writes=[c.sgb])
                op("act", lambda e: e.activation(out=c.sg[:, 0:128], in_=c.sg[:, 0:128], func=AF.Exp, scale=-1.0), reads=[c.sgb], writes=[c.sgb])
                c.vo, c.vob = r_vo.get()
                op("act", lambda e: e.activation(out=c.vo[:], in_=c.P[:, 128:256], func=AF.Copy), reads=[c.Pb], writes=[c.vob])

            def a2(c):
                c.omf, c.omfb = r_omf.get()
                op("dve", lambda e: e.tensor_tensor(out=c.omf[:, 0:128], in0=c.sg[:, 0:128], in1=omlt[:, 1, hs], op=ALU.mult),
                   reads=[c.sgb, omltb], writes=[c.omfb])

            def a3(c):
                c.lf, c.lfb = r_lf.get()
                op("act", lambda e: e.activation(out=c.lf[:, 0:128], in_=c.omf[:, 0:128], func=AF.Ln, scale=-1.0, bias=1.0),
                   reads=[c.omfb], writes=[c.lfb])

            def a4(c):
                c.Eb, c.Ebb = g.bank_from("E")
                mm_group(g, c.Eb[:, 0:128], c.Ebb, [(m3[1], c.lf[:, 0:128])], reads=[cstb, c.lfb])
                mm_group(g, c.Eb[:, 128:132], c.Ebb, [(c.lf[:, 0:128], mk[1][:, 256:260])], reads=[cstb, c.lfb])

            def a5(c):
                c.ex3, c.ex3b = r_ex3.get()
                op("act", lambda e: e.activation(out=c.ex3[:, 0:128], in_=c.Eb[:, 0:128], func=AF.Exp), reads=[c.Ebb], writes=[c.ex3b])
                op("act", lambda e: e.activation(out=dec[1][:, c.c0:c.c0 + 4], in_=c.Eb[:, 128:132], func=AF.Exp),
                   reads=[c.Ebb], writes=[decb[1]])

            def a6(c):
                c.khm, c.khmb = r_khm.get()
                for k in range(4):
                    op("dve", lambda e, k=k: e.scalar_tensor_tensor(out=c.khm[:, 0, k, :], in0=c.ex3[:, 0:128], scalar=cmask(1, k),
                                                                    in1=c.omf[:, 0:128], op0=ALU.mult, op1=ALU.mult),
                       reads=[c.ex3b, c.omfb, cstb], writes=[c.khmb])

            def a7(c):
                c.Kp, c.Kpb = g.bank_from("K")
                for k in range(4):
                    mm_group(g, c.Kp[:, k * 128:(k + 1) * 128], c.Kpb, [(c.khm[:, 0, k, :], c.vo[:])], reads=[c.khmb, c.vob])
                if c.T % 2 == 0:
                    op("act", lambda e: e.activation(out=kv[1][:, :, c.c0:c.c0 + 4].rearrange("p e c -> p c e"),
                                                     in_=c.Kp[:].rearrange("p (c e) -> p c e", e=128),
                                                     func=AF.Copy), reads=[c.Kpb], writes=[kvb[1]])
                else:
                    op("dve", lambda e: e.tensor_copy(out=kv[1][:, :, c.c0:c.c0 + 4].rearrange("p e c -> p c e"),
                                                      in_=c.Kp[:].rearrange("p (c e) -> p c e", e=128)), reads=[c.Kpb], writes=[kvb[1]])

            run_pipeline(list(range(16, 32)), [a0, a1, a2, a3, a4, a5, a6, a7])
            scan_all(1, fold=False)
            op("act", lambda e: e.activation(out=bound[:], in_=kv[1][:, :, 63], func=AF.Copy), reads=[kvb[1]], writes=[boundb])
            op("act", lambda e: e.activation(out=bnd16[:], in_=kv[1][:, :, 63], func=AF.Copy), reads=[kvb[1]], writes=[bnd16b])

            blk = {}

            def b0(c):
                T = c.T
                B, tt = T // 4, T % 4
                if tt == 0:
                    qT, qTb = r_qT.get()
                    sgT, sgTb = r_sgT.get()
                    for cb in [0, 3, 1, 2]:
                        Pc, Pcb = g.bank_from("K")
                        mm_group(g, Pc[:], Pcb, [(w_cm[:, k, cb * 128:(cb + 1) * 128], xTo[:, k, B * 512:(B + 1) * 512]) for k in range(8)],
                                 reads=[wcmb, xTob[B]])
                        if cb == 0:
                            op("act", lambda e: e.activation(out=qT[:], in_=Pc[:], func=AF.Silu), reads=[Pcb], writes=[qTb])
                        elif cb == 3:
                            op("act", lambda e: e.activation(out=sgl[:, B * 512:(B + 1) * 512], in_=Pc[:], func=AF.Silu),
                               reads=[Pcb], writes=[sglb[B]])
                        else:
                            op("act", lambda e, cb=cb: e.activation(out=sgT[:, cb - 1, :], in_=Pc[:], func=AF.Sigmoid, scale=-1.0),
                               reads=[Pcb], writes=[sgTb])
                    blk[B] = (qT, qTb, sgT, sgTb)
                c.tc = slice(T * 128, (T + 1) * 128)
                c.tl = slice(tt * 128, (tt + 1) * 128)
                c.qT, c.qTb, c.sgT, c.sgTb = blk[B]
                c.P, c.Pb = g.bank_from("P")
                mm_group(g, c.P[:, 0:384], c.Pb, [(xTo[:, k, c.tc], w_tm[:, k, 0:384]) for k in range(8)], reads=[xTob[B], wtmb])

            def b1(c):
                c.sg, c.sgb = r_sg.get()
                op("act", lambda e: e.activation(out=c.sg[:], in_=c.P[:, 0:256], func=AF.Exp), reads=[c.Pb], writes=[c.sgb])
                op("act", lambda e: e.activation(out=c.sg[:], in_=c.sg[:], func=AF.Ln, bias=1.0), reads=[c.sgb], writes=[c.sgb])
                op("act", lambda e: e.activation(out=c.sg[:], in_=c.sg[:], func=AF.Exp, scale=-1.0), reads=[c.sgb], writes=[c.sgb])
                op("act", lambda e: e.activation(out=V[:, c.T, :], in_=c.P[:, 256:384], func=AF.Copy), reads=[c.Pb], writes=[Vb[c.T]])

            def b2(c):
                c.omf, c.omfb = r_omf.get()
                op("dve", lambda e: e.tensor_tensor(out=c.omf[:].rearrange("p (a d) -> p a d", a=2),
                                                    in0=c.sg[:].rearrange("p (a d) -> p a d", a=2),
                                                    in1=omlt[:, :, hs], op=ALU.mult),
                   reads=[c.sgb, omltb], writes=[c.omfb])

            def b3(c):
                c.lf, c.lfb = r_lf.get()
                op("act", lambda e: e.activation(out=c.lf[:], in_=c.omf[:], func=AF.Ln, scale=-1.0, bias=1.0), reads=[c.omfb], writes=[c.lfb])

            def b4(c):
                c.E, c.Eb_ = g.bank_from("E")
                c.E3, c.E3b = g.bank_from("E")
                for dr in range(2):
                    ds = slice(dr * 128, (dr + 1) * 128)
                    mm_group(g, c.E[:, dr * 256:(dr + 1) * 256], c.Eb_, [(c.lf[:, ds], mk[dr][:, 0:256])], reads=[cstb, c.lfb])
                    mm_group(g, c.E3[:, ds], c.E3b, [(m3[dr], c.lf[:, ds])], reads=[cstb, c.lfb])
                    mm_group(g, c.E3[:, 256 + 4 * dr:260 + 4 * dr], c.E3b, [(c.lf[:, ds], mk[dr][:, 256:260])], reads=[cstb, c.lfb])

            def b5(c):
                T = c.T
                c.ex3, c.ex3b = r_ex3.get()
                op("act", lambda e: e.activation(out=c.ex3[:], in_=c.E3[:, 0:256], func=AF.Exp), reads=[c.E3b], writes=[c.ex3b])
                c.ex12, c.ex12b = r_ex12.get()
                op("act", lambda e: e.activation(out=c.ex12[:].rearrange("p a t -> p (a t)"), in_=c.E[:, 0:512], func=AF.Exp),
                   reads=[c.Eb_], writes=[c.ex12b])
                c.exm, c.exmb = r_exm.get()
                op("act", lambda e: e.activation(out=c.exm[:], in_=c.E[:, 0:512].rearrange("p (a t) -> p a t", a=2)[:, :, 0:128],
                                                 func=AF.Exp, scale=-1.0), reads=[c.Eb_], writes=[c.exmb])
                for dr in range(2):
                    c0 = 4 * T if dr == 0 else 60 - 4 * T
                    op("act", lambda e, dr=dr, c0=c0: e.activation(out=dec[dr][:, c0:c0 + 4], in_=c.E3[:, 256 + 4 * dr:260 + 4 * dr], func=AF.Exp),
                       reads=[c.E3b], writes=[decb[dr]])

            def b6(c):
                T = c.T
                c.khm, c.khmb = r_khm.get()
                for dr in range(2):
                    ds = slice(dr * 128, (dr + 1) * 128)
                    op("pool", lambda e, dr=dr: e.tensor_tensor(out=big[("qt", dr)][:, c.tc], in0=c.qT[:, c.tl], in1=c.ex12[:, dr, 0:128], op=ALU.mult),
                       reads=[c.qTb, c.ex12b], writes=[bigb[("qt", dr)][T]])
                    op("pool", lambda e, dr=dr: e.tensor_tensor(out=big[("Qh", dr)][:, c.tc], in0=c.qT[:, c.tl], in1=c.ex12[:, dr, 128:256], op=ALU.mult),
                       reads=[c.qTb, c.ex12b], writes=[bigb[("Qh", dr)][T]])
                    op("dve", lambda e, dr=dr: e.scalar_tensor_tensor(out=big[("kt", dr)][:, c.tc], in0=c.sgT[:, dr, c.tl],
                                                                      scalar=omlc[:, dr, h:h + 1], in1=c.exm[:, dr, :], op0=ALU.mult, op1=ALU.mult),
                       reads=[c.sgTb, omlcb, c.exmb], writes=[bigb[("kt", dr)][T]])
                    for k in range(4):
                        op("dve", lambda e, k=k, dr=dr, ds=ds: e.scalar_tensor_tensor(out=c.khm[:, dr, k, :], in0=c.ex3[:, ds], scalar=cmask(dr, k),
                                                                                      in1=c.omf[:, ds], op0=ALU.mult, op1=ALU.mult),
                           reads=[c.ex3b, c.omfb, cstb], writes=[c.khmb])

            def b7(c):
                T = c.T
                for dr in range(2):
                    Kp, Kpb = g.bank_from("K")
                    for k in range(4):
                        mm_group(g, Kp[:, k * 128:(k + 1) * 128], Kpb, [(c.khm[:, dr, k, :], V[:, c.T, :])], reads=[c.khmb, Vb[c.T]])
                    c0 = 4 * T if dr == 0 else 60 - 4 * T
                    if dr == 0:
                        op("act", lambda e, c0=c0, dr=dr, Kp=Kp: e.activation(out=kv[dr][:, :, c0:c0 + 4].rearrange("p e c -> p c e"),
                                                                              in_=Kp[:].rearrange("p (c e) -> p c e", e=128),
                                                                              func=AF.Copy), reads=[Kpb, boundb, bnd16b], writes=[kvb[dr]])
                    else:
                        op("dve", lambda e, c0=c0, dr=dr, Kp=Kp: e.tensor_copy(out=kv[dr][:, :, c0:c0 + 4].rearrange("p e c -> p c e"),
                                                                               in_=Kp[:].rearrange("p (c e) -> p c e", e=128)),
                           reads=[Kpb, boundb, bnd16b], writes=[kvb[dr]])

            run_pipeline(list(range(NT)), [b0, b1, b2, b3, b4, b5, b6, b7])
            if h + 1 < 4:
                xpre = load_head(h + 1)
            scan_all(0, fold=False)
            scan_all(1, fold=True)
            obk = {}

            def d0(c):
                T = c.T
                c.tc = slice(T * 128, (T + 1) * 128)
                c.S, c.Sb = g.bank_from("P")
                for dr in range(2):
                    mm_group(g, c.S[:, dr * 128:(dr + 1) * 128], c.Sb, [(big[("kt", dr)][:, c.tc], big[("qt", dr)][:, c.tc])],
                             reads=[bigb[("kt", dr)][T], bigb[("qt", dr)][T]])

            def d1(c):
                c.scm, c.scmb = r_scm.get()
                op("dve", lambda e: e.tensor_tensor(out=c.scm[:].rearrange("p (a t) -> p a t", a=2),
                                                    in0=c.S[:, 0:256].rearrange("p (a t) -> p a t", a=2),
                                                    in1=cst[:, 0:520].rearrange("p (a t) -> p a t", a=2)[:, :, 128:256],
                                                    op=ALU.mult), reads=[c.Sb, cstb], writes=[c.scmb])

            def d2(c):
                T = c.T
                B, tt = T // 4, T % 4
                if tt == 0:
                    obk[B] = g.bank_from("E")
                O, Ob = obk[B]
                oc = O[:, tt * 128:(tt + 1) * 128]
                rd = [Vb[T], c.scmb, kvb[0], kvb[1], bnd16b, bigb[("Qh", 0)][T], bigb[("Qh", 1)][T]]
                seq = [(V[:, T, :], c.scm[:, 0:128], oc, True), (V[:, T, :], c.scm[:, 128:256], oc, False)]
                for cq in range(4):
                    j = 4 * T + cq
                    occ = O[:, tt * 128 + cq * 32:tt * 128 + cq * 32 + 32]
                    qs = slice(T * 128 + cq * 32, T * 128 + cq * 32 + 32)
                    ib = 63 - j
                    sb_ap = kv[1][:, :, ib - 1] if ib >= 1 else bnd16[:]
                    seq.append((sb_ap, big[("Qh", 1)][:, qs], occ, False))
                    if j >= 1:
                        seq.append((kv[0][:, :, j - 1], big[("Qh", 0)][:, qs], occ, False))
                n = len(seq)
                for i, (l, r, o_ap, st) in enumerate(seq):
                    op("pe", lambda e, l=l, r=r, o_ap=o_ap, i=i, st=st: e.matmul(o_ap, lhsT=l, rhs=r, start=st, stop=(i == n - 1),
                                                                                 skip_group_check=True),
                       reads=rd, writes=[Ob], signal=(i == n - 1))
                if tt == 3:
                    osq, osqb = r_f512.get()
                    op("act", lambda e: e.activation(out=osq[:], in_=O[:], func=AF.Square), reads=[Ob], writes=[osqb])
                    Q, Qb = g.bank_from("K")
                    mm_group(g, Q[:], Qb, [(ones, osq[:])], reads=[cstb, osqb])
                    rt, rtb = r_f512.get()
                    op("act", lambda e: e.activation(out=rt[:], in_=Q[:], func=AF.Sqrt, scale=1.0 / 128.0, bias=RMS_EPS), reads=[Qb], writes=[rtb])
                    op("dve", lambda e: e.reciprocal(out=rt[:], in_=rt[:]), reads=[rtb], writes=[rtb])
                    t1, t1b = r_f512.get()
                    op("dve", lambda e: e.scalar_tensor_tensor(out=t1[:], in0=O[:], scalar=nrm[:, h:h + 1], in1=rt[:], op0=ALU.mult, op1=ALU.mult),
                       reads=[Ob, nrmb, rtb], writes=[t1b])
                    op("pool", lambda e: e.tensor_tensor(out=ybT[:, h, B * 512:(B + 1) * 512], in0=t1[:], in1=sgl[:, B * 512:(B + 1) * 512],
                                                         op=ALU.mult), reads=[t1b, sglb[B]], writes=[ybb[h][B]])

            run_pipeline(list(range(NT)), [d0, d1, d2])
        if stage == "mixer_dbg":
            g.dbg_evs2 = []
            kb.barrier()
            for nm, t_ in [("qt0", big[("qt", 0)]), ("Qh0", big[("Qh", 0)]), ("kt0", big[("kt", 0)]), ("qt1", big[("qt", 1)])]:
                dd = nc.dram_tensor("dbg_" + nm, [128, HALF], BF16, kind="ExternalOutput").ap()
                g.dbg_evs2.append(kb.dma("sp", dd[:, :], t_[:]))
            for nm, t_ in [("kv0", kv[0]), ("kv1", kv[1])]:
                dd = nc.dram_tensor("dbg_" + nm, [128, 128, 64], BF16, kind="ExternalOutput").ap()
                g.dbg_evs2.append(kb.dma("sp", dd[:, :, :], t_[:]))
            dd = nc.dram_tensor("dbg_dec0", [128, 64], F32, kind="ExternalOutput").ap()
            g.dbg_evs2.append(kb.dma("sp", dd[:, :], dec[0][:]))
            dd = nc.dram_tensor("dbg_V", [128, NT, 128], BF16, kind="ExternalOutput").ap()
            g.dbg_evs2.append(kb.dma("sp", dd[:, :, :], V[:]))
        kb.barrier()

    with ExitStack() as es:
        al = allocator(nc, es)
        cvw = al("cvw", [128, 4, 3], F32)
        cvwb = Buf()
        kb.dma("sp", cvw[:], g.convw.rearrange("(h p) c -> p h c", p=128), writes=[cvwb])
        w_cv = Ring(al, "w_cv", [128, 8, 384], BF16, 2)
        zr = Ring(al, "z", [128, HALF + 2], F32, 2)
        cbsr = Ring(al, "cbs", [128, HALF], F32, 2)
        cvar = Ring(al, "cva", [128, HALF], F32, 2)
        r_f512 = Ring(al, "c_f512", [128, 512], F32, 3)
        for i in range(2):
            op("pool", lambda e, i=i: e.memset(zr.t[i][:, 0:1], 0.0), writes=[zr.b[i]])
        for cb in range(4):
            wt, wtb = w_cv.get()
            z, zb = zr.get()
            cbs, cbsb = cbsr.get()
            cva, cvab = cvar.get()
            for i, c0 in enumerate([C_B, C_C, C_H]):
                w_dma(g, wt[:, :, i * 128:(i + 1) * 128], g.w_in[:, c0 + cb * 128:c0 + (cb + 1) * 128], wtb)
            for B in range(5):
                cols = slice(B * 512, (B + 1) * 512) if B < 4 else slice(HALF, HALF + 8)
                ncol = 512 if B < 4 else 8
                ps = []
                for i in range(3):
                    if B == 4 and i == 0:
                        ps.append(None)
                        continue
                    Pc, Pcb = g.bank()
                    mm_group(g, Pc[:, 0:ncol], Pcb, [(wt[:, k, i * 128:(i + 1) * 128], xTo[:, k, cols]) for k in range(8)],
                             reads=[wtb, xTob[B]])
                    ps.append((Pc, Pcb))
                tmp, tmpb = r_f512.get()
                op("act", lambda e: e.activation(out=tmp[:, 0:ncol], in_=ps[1][0][:, 0:ncol], func=AF.Copy), reads=[ps[1][1]], writes=[tmpb])
                if B < 4:
                    op("dve", lambda e: e.tensor_tensor(out=z[:, 1 + B * 512:1 + (B + 1) * 512], in0=tmp[:], in1=ps[2][0][:], op=ALU.mult),
                       reads=[tmpb, ps[2][1]], writes=[zb])
                    op("act", lambda e: e.activation(out=cbs[:, cols], in_=ps[0][0][:], func=AF.Copy), reads=[ps[0][1]], writes=[cbsb])
                else:
                    op("dve", lambda e: e.tensor_tensor(out=z[:, HALF + 1:HALF + 2], in0=tmp[:, 0:1], in1=ps[2][0][:, 0:1], op=ALU.mult),
                       reads=[tmpb, ps[2][1]], writes=[zb])
            op("dve", lambda e: e.tensor_scalar(out=cva[:], in0=z[:, 0:HALF], scalar1=cvw[:, cb, 0:1], scalar2=None, op0=ALU.mult),
               reads=[zb, cvwb], writes=[cvab])
            op("dve", lambda e: e.scalar_tensor_tensor(out=cva[:], in0=z[:, 1:HALF + 1], scalar=cvw[:, cb, 1:2], in1=cva[:],
                                                       op0=ALU.mult, op1=ALU.add), reads=[zb, cvwb, cvab], writes=[cvab])
            op("dve", lambda e: e.scalar_tensor_tensor(out=cva[:], in0=z[:, 2:HALF + 2], scalar=cvw[:, cb, 2:3], in1=cva[:],
                                                       op0=ALU.mult, op1=ALU.add), reads=[zb, cvwb, cvab], writes=[cvab])
            op("pool", lambda e: e.tensor_tensor(out=yaT[:, cb, :], in0=cva[:], in1=cbs[:], op=ALU.mult),
               reads=[cvab, cbsb], writes=[yab[cb]])
        kb.barrier()

    if stage == "mixer_dbg":
        dya = nc.dram_tensor("dbg_ya", [128, 4, HALF], BF16, kind="ExternalOutput").ap()
        dyb = nc.dram_tensor("dbg_yb", [128, 4, HALF], BF16, kind="ExternalOutput").ap()
        g.dbg_evs = [kb.dma("sp", dya[:, :, :], yaT[:], reads=yab),
                     kb.dma("sp", dyb[:, :, :], ybT[:], reads=[ybb[hh][B] for hh in range(4) for B in range(4)])]
    with ExitStack() as es:
        al = allocator(nc, es)
        pa = al("pa", [128, 4, D], BF16)
        pb_ = al("pb", [128, 4, D], BF16)
        wo = al("wo", [128, 8, D], BF16)
        wga = al("wga", [128, 8, D], BF16)
        wgb = al("wgb", [128, 8, D], BF16)
        pab, pbb, wob, wgab, wgbb = Buf(), Buf(), Buf(), Buf(), Buf()
        w_dma(g, pa[:], g.p_a, pab)
        w_dma(g, pb_[:], g.p_b, pbb)
        w_dma(g, wo[:], g.w_o, wob)
        w_dma(g, wga[:], g.w_in[:, C_GA:C_GA + D], wgab)
        w_dma(g, wgb[:], g.w_in[:, C_GB:C_GB + D], wgbb)
        lng = al("lng", [128, D], F32)
        lnb = al("lnb", [128, D], F32)
        lngb, lnbb = Buf(), Buf()
        kb.dma("sp", lng[:], g.ln1g[0:1, :].broadcast_to([128, D]), writes=[lngb])
        kb.dma("sp", lnb[:], g.ln1b[0:1, :].broadcast_to([128, D]), writes=[lnbb])
        mixT = Ring(al, "mixT", [128, 8, 512], BF16, 2)
        r_x = Ring(al, "r_x", [128, D], F32, 3)
        r_r = Ring(al, "r_r", [128, D], F32, 4)
        r_st = Ring(al, "r_st", [128, 8], F32, 4)
        r_f512 = Ring(al, "g_f512", [128, 512], F32, 4)
        g.h_evs = []
        allyb = [ybb[hh][B] for hh in range(4) for B in range(4)]
        hdst = g.out if stage.startswith("mixer") else g.hs
        for B in range(4):
            bc = slice(B * 512, (B + 1) * 512)
            mt, mtb = mixT.get()
            for mc in range(8):
                ms = slice(mc * 128, (mc + 1) * 128)
                A_, Ab = g.bank()
                mm_group(g, A_[:], Ab, [(pa[:, k, ms], yaT[:, k, bc]) for k in range(4)], reads=[pab] + yab)
                B_, Bb = g.bank()
                mm_group(g, B_[:], Bb, [(pb_[:, k, ms], ybT[:, k, bc]) for k in range(4)], reads=[pbb] + allyb)
                GA, GAb = g.bank()
                mm_group(g, GA[:], GAb, [(wga[:, k, ms], xTo[:, k, bc]) for k in range(8)], reads=[wgab, xTob[B]])
                GB, GBb = g.bank()
                mm_group(g, GB[:], GBb, [(wgb[:, k, ms], xTo[:, k, bc]) for k in range(8)], reads=[wgbb, xTob[B]])
                sa, sab = r_f512.get()
                op("act", lambda e: e.activation(out=sa[:], in_=GA[:], func=AF.Sigmoid), reads=[GAb], writes=[sab])
                sb2, sbb = r_f512.get()
                op("act", lambda e: e.activation(out=sb2[:], in_=GB[:], func=AF.Sigmoid), reads=[GBb], writes=[sbb])
                op("dve", lambda e: e.tensor_tensor(out=sa[:], in0=sa[:], in1=A_[:], op=ALU.mult), reads=[sab, Ab], writes=[sab])
                op("dve", lambda e: e.tensor_tensor(out=sb2[:], in0=sb2[:], in1=B_[:], op=ALU.mult), reads=[sbb, Bb], writes=[sbb])
                op("pool", lambda e: e.tensor_tensor(out=mt[:, mc, :], in0=sa[:], in1=sb2[:], op=ALU.add), reads=[sab, sbb], writes=[mtb])
            for tt in range(4):
                T = 4 * B + tt
                xt_, xtb = r_x.get()
                kb.dma("sp", xt_[:], g.xo[T * 128:(T + 1) * 128, :], writes=[xtb])
                r_, rb = r_r.get()
                for hf in range(2):
                    O, Ob = g.bank()
                    mm_group(g, O[:], Ob, [(mt[:, k, tt * 128:(tt + 1) * 128], wo[:, k, hf * 512:(hf + 1) * 512]) for k in range(8)],
                             reads=[mtb, wob])
                    op("dve", lambda e: e.scalar_tensor_tensor(out=r_[:, hf * 512:(hf + 1) * 512], in0=xt_[:, hf * 512:(hf + 1) * 512],
                                                               scalar=ALPHA, in1=O[:], op0=ALU.mult, op1=ALU.add),
                       reads=[xtb, Ob], writes=[rb])
                layer_norm(g, r_, rb, xt_, xtb, lng, lngb, lnb, lnbb, r_st)
                ev = kb.dma("sp", hdst[T * 128:(T + 1) * 128, :], xt_[:], reads=[xtb])
                g.h_evs.append(ev)
        kb.barrier()
    g.out_evs = list(g.h_evs)


def layer_norm(g, r_, rb, o_, ob, lng, lngb, lnb, lnbb, r_st, last="pool", gmul="dve"):
    kb = g.kb
    op = kb.op
    st, stb = r_st.get()
    op("act", lambda e: e.activation(out=o_[:], in_=r_[:], func=AF.Identity, accum_out=st[:, 0:1]), reads=[rb], writes=[ob, stb])
    op("dve", lambda e: e.tensor_scalar(out=st[:, 1:2], in0=st[:, 0:1], scalar1=-1.0 / D, scalar2=None, op0=ALU.mult),
       reads=[stb], writes=[stb])
    op("act", lambda e: e.activation(out=o_[:], in_=r_[:], func=AF.Square, bias=st[:, 1:2], accum_out=st[:, 2:3]),
       reads=[rb, stb], writes=[ob, stb])
    op("act", lambda e: e.activation(out=st[:, 3:4], in_=st[:, 2:3], func=AF.Sqrt, scale=1.0 / D, bias=LN_EPS), reads=[stb], writes=[stb])
    op("dve", lambda e: e.reciprocal(out=st[:, 4:5], in_=st[:, 3:4]), reads=[stb], writes=[stb])
    op("dve", lambda e: e.tensor_scalar(out=r_[:], in0=r_[:], scalar1=st[:, 1:2], scalar2=st[:, 4:5], op0=ALU.add, op1=ALU.mult),
       reads=[rb, stb], writes=[rb])
    op(gmul, lambda e: e.tensor_tensor(out=r_[:], in0=r_[:], in1=lng[:], op=ALU.mult), reads=[rb, lngb], writes=[rb])
    return op(last, lambda e: e.tensor_tensor(out=o_[:], in0=r_[:], in1=lnb[:], op=ALU.add), reads=[rb, lnbb], writes=[ob])


def moe(g, stage="full"):
    if DBG.get("dense", 0):
        return moe_dense(g, stage)
    ne_run = ne_decl(stage)
    nc, kb = g.nc, g.kb
    cst, cstb = g.cst, g.cstb
    op = kb.op
    ident = cst[:, K_ID:K_ID + 128]
    I32 = mybir.dt.int32
    NR = NE * CAP
    Xg = nc.dram_tensor("xg_scratch", [NR, D], BF16, kind="Internal").ap()
    Yg = nc.dram_tensor("yg_scratch", [NR, D], BF16, kind="Internal").ap()
    xgb, ygb = Buf(), Buf()
    with ExitStack() as es:
        al = allocator(nc, es)
        didx = al("didx", [128, NT, 4], I32)
        didxb = [Buf() for _ in range(NT)]
        gk = al("gk", [128, NT, 4], F32)
        gkb = [Buf() for _ in range(NT)]
        onesb = al("onesb", [128, 128], BF16)
        ltb = al("ltb", [128, 128], BF16)
        cb16 = Buf()
        op("dve", lambda e: e.tensor_copy(out=onesb[:], in_=cst[:, K_ONE:K_ONE + 128]), reads=[cstb], writes=[cb16])
        op("dve", lambda e: e.tensor_copy(out=ltb[:], in_=cst[:, K_LT:K_LT + 128]), reads=[cstb], writes=[cb16])

        with ExitStack() as es2:
            al2 = allocator(nc, es2)
            zt = al2("zt", [128, 8 * D], BF16)
            ztb = Buf()
            op("pool", lambda e: e.memset(zt[:], 0.0), writes=[ztb])
            xgv = Xg.rearrange("(n p j) d -> n p (j d)", p=128, j=8)
            for n in range(NR // 1024):
                kb.dma("sp", xgv[n], zt[:], reads=[ztb], writes=[xgb])
            wr = al2("wr", [128, 8, NE], F32)
            wrb = Buf()
            kb.dma("sp", wr[:], g.w_r.rearrange("(k p) n -> p k n", p=128), writes=[wrb])
            br = al2("br", [1, NE], F32)
            brb = Buf()
            kb.dma("sp", br[:], g.b_r[0:1, :], writes=[brb])
            msum = al2("msum", [128, NE], BF16)
            msumb = Buf()
            op("pool", lambda e: e.memset(msum[:], 0.0), writes=[msumb])
            r_h = Ring(al2, "r_h", [128, D], F32, 2)
            r_hb = Ring(al2, "r_hb", [128, D], BF16, 7)
            r_h32 = Ring(al2, "r_h32", [128, 8, 128], F32, 2)
            r_lg = Ring(al2, "r_lg", [128, 4, NE], F32, 3)
            r_mk = Ring(al2, "r_mk", [128, NE], BF16, 2)
            r_sm = Ring(al2, "r_sm", [128, 24], F32, 3)
            def r0(c):
                T = c.T
                c.ht, c.htb = r_h.get()
                kb.dma("sp", c.ht[:], g.hs[T * 128:(T + 1) * 128, :], writes=[c.htb])
                c.hb, c.hbb = r_hb.get()
                op("act", lambda e: e.activation(out=c.hb[:], in_=c.ht[:], func=AF.Copy), reads=[c.htb], writes=[c.hbb])
                c.Pt = []
                for half in range(2):
                    Pt, Ptb = g.bank_from("E")
                    for kk in range(4):
                        k = half * 4 + kk
                        op("pe", lambda e, k=k, kk=kk, Pt=Pt: e.transpose(out=Pt[:, kk * 128:(kk + 1) * 128], in_=c.ht[:, k * 128:(k + 1) * 128],
                                                                          identity=ident), reads=[c.htb, cstb], writes=[Ptb], signal=(kk == 3))
                    c.Pt.append((Pt, Ptb))

            def r1(c):
                c.h32, c.h32b = r_h32.get()
                Pt, Ptb = c.Pt[0]
                op("act", lambda e: e.activation(out=c.h32[:, 0:4, :], in_=Pt[:].rearrange("p (k t) -> p k t", k=4), func=AF.Copy),
                   reads=[Ptb], writes=[c.h32b])
                Pt, Ptb = c.Pt[1]
                op("dve", lambda e: e.tensor_copy(out=c.h32[:, 4:8, :], in_=Pt[:].rearrange("p (k t) -> p k t", k=4)),
                   reads=[Ptb], writes=[c.h32b])

            def r2(c):
                c.L, c.Lb = g.bank_from("P")
                pairs = [(c.h32[:, k, :], wr[:, k, :]) for k in range(8)] + [(cst[0:1, K_ONE:K_ONE + 128], br[0:1, :])]
                mm_group(g, c.L[:, 0:NE], c.Lb, pairs, reads=[c.h32b, wrb, brb, cstb])

            def r3(c):
                c.lg, c.lgb = r_lg.get()
                c.sm, c.smb = r_sm.get()
                c.mk_, c.mkb = r_mk.get()
                lg, lgb, sm, smb, mk_, mkb = c.lg, c.lgb, c.sm, c.smb, c.mk_, c.mkb
                op("dve", lambda e: e.tensor_copy(out=lg[:, 0, :], in_=c.L[:, 0:NE]), reads=[c.Lb], writes=[lgb])
                op("dve", lambda e: e.max(out=sm[:, 0:8], in_=lg[:, 0, :]), reads=[lgb], writes=[smb])
                op("dve", lambda e: e.tensor_scalar(out=mk_[:], in0=lg[:, 0, :], scalar1=sm[:, 3:4], scalar2=None, op0=ALU.is_ge),
                   reads=[lgb, smb], writes=[mkb])

            def r4(c):
                c.Pp, c.Ppb = g.bank_from("K")
                mm_group(g, c.Pp[:, 0:NE], c.Ppb, [(ltb[:], c.mk_[:]), (onesb[:], msum[:])], reads=[cb16, c.mkb, msumb])
                op("pool", lambda e: e.tensor_tensor(out=msum[:], in0=msum[:], in1=c.mk_[:], op=ALU.add), reads=[c.mkb, msumb], writes=[msumb])

            def r5(c):
                T = c.T
                lg, lgb, sm, smb = c.lg, c.lgb, c.sm, c.smb
                Pp, Ppb = c.Pp, c.Ppb
                op("dve", lambda e: e.tensor_tensor(out=lg[:, 1, :], in0=Pp[:, 0:NE], in1=cst[:, K_EC:K_EC + NE], op=ALU.add),
                   reads=[Ppb, cstb], writes=[lgb])
                op("dve", lambda e: e.tensor_scalar(out=lg[:, 2, :], in0=Pp[:, 0:NE], scalar1=float(CAP), scalar2=1.0e7, op0=ALU.is_ge, op1=ALU.mult),
                   reads=[Ppb], writes=[lgb])
                op("dve", lambda e: e.tensor_tensor(out=lg[:, 1, :], in0=lg[:, 1, :], in1=lg[:, 2, :], op=ALU.add), reads=[lgb], writes=[lgb])
                for k in range(4):
                    op("dve", lambda e, k=k: e.scalar_tensor_tensor(out=lg[:, 3, :], in0=lg[:, 0, :], scalar=sm[:, k:k + 1], in1=lg[:, 1, :],
                                                                    op0=ALU.is_equal, op1=ALU.mult, accum_out=sm[:, 8 + k:9 + k]),
                       reads=[lgb, smb], writes=[lgb, smb])
                op("dve", lambda e: e.tensor_copy(out=didx[:, T, :], in_=sm[:, 8:12]), reads=[smb], writes=[didxb[T]])
                op("dve", lambda e: e.tensor_scalar(out=sm[:, 12:13], in0=sm[:, 0:1], scalar1=-1.0, scalar2=None, op0=ALU.mult),
                   reads=[smb], writes=[smb])
                op("act", lambda e: e.activation(out=sm[:, 13:17], in_=sm[:, 0:4], func=AF.Exp, bias=sm[:, 12:13], accum_out=sm[:, 17:18]),
                   reads=[smb], writes=[smb])
                op("dve", lambda e: e.reciprocal(out=sm[:, 18:19], in_=sm[:, 17:18]), reads=[smb], writes=[smb])
                op("dve", lambda e: e.tensor_scalar(out=sm[:, 19:23], in0=sm[:, 8:12], scalar1=1.0e6, scalar2=None, op0=ALU.is_lt),
                   reads=[smb], writes=[smb])
                op("dve", lambda e: e.scalar_tensor_tensor(out=gk[:, T, :], in0=sm[:, 13:17], scalar=sm[:, 18:19], in1=sm[:, 19:23],
                                                           op0=ALU.mult, op1=ALU.mult), reads=[smb], writes=[gkb[T]])
                for k in range(4):
                    kb.idma(Xg[:, :], c.hb[:, :], didx[:, T, k:k + 1], True, NR - 1, reads=[c.hbb, didxb[T]], writes=[xgb])

            stages = [r0, r1, r2, r3, r4, r5]
            cs = []
            for T in range(NT):
                c = Ctx()
                c.T = T
                cs.append(c)
            for step in range(NT + len(stages) - 1):
                for si in range(len(stages)):
                    i = step - si
                    if 0 <= i < NT:
                        stages[si](cs[i])
            kb.barrier()

        with ExitStack() as es2:
            al2 = allocator(nc, es2)
            wch = Ring(al2, "wch", [128, 8, 512], BF16, 12)
            r_b1 = Ring(al2, "r_b1", [128, 16], F32, 3)
            r_b2 = Ring(al2, "r_b2", [1, D], BF16, 3)
            r_xg = Ring(al2, "r_xg", [128, CAP // 128, D], BF16, 2)
            r_xT = Ring(al2, "r_xT", [128, 8, CAP], BF16, 2)
            r_aT = Ring(al2, "r_aT", [128, 8, CAP], BF16, 2)
            r_hg = Ring(al2, "r_hg", [128, CAP], F32, 2)
            r_sg = Ring(al2, "r_sgm", [128, CAP], BF16, 2)
            r_hl = Ring(al2, "r_hl", [128, CAP], F32, 2)
            r_a1 = Ring(al2, "r_a1", [128, CAP], BF16, 2)
            r_y = Ring(al2, "r_y", [128, D], BF16, 3)
            NJ = CAP // 128
            def prep(ex):
                st = Ctx()
                ch = []
                for q in [0, 2, 1, 3]:
                    t_, b_ = wch.get()
                    w_dma(g, t_[:], g.w1[ex, :, q * 512:(q + 1) * 512], b_)
                    ch.append((q, t_, b_))
                st.w1c = {q: (t_, b_) for q, t_, b_ in ch}
                st.w2c = []
                for half in range(2):
                    t_, b_ = wch.get()
                    w_dma(g, t_[:], g.w2[ex, :, half * 512:(half + 1) * 512], b_)
                    st.w2c.append((t_, b_))
                st.b1, st.b1b = r_b1.get()
                kb.dma("sp", st.b1[:], g.b1[ex, :, :], writes=[st.b1b])
                st.b2, st.b2b = r_b2.get()
                kb.dma("pool", st.b2[:], g.b2[ex:ex + 1, :], writes=[st.b2b])
                st.xg, st.xgtb = r_xg.get()
                kb.dma("sp", st.xg[:], Xg[ex * CAP:(ex + 1) * CAP, :].rearrange("(j p) d -> p j d", p=128), reads=[xgb], writes=[st.xgtb])
                return st

            def transposes(st):
                xg, xgtb = st.xg, st.xgtb
                st.xT, st.xTb = r_xT.get()
                xT, xTb = st.xT, st.xTb
                for kp in range(4):
                    Pt, Ptb = g.bank()
                    Pv = Pt[:].bitcast(BF16)
                    n_tr = 2 * NJ
                    i = 0
                    for kk in range(2):
                        for j in range(NJ):
                            k = 2 * kp + kk
                            i += 1
                            op("pe", lambda e, k=k, kk=kk, j=j: e.transpose(out=Pv[:, kk * CAP + j * 128:kk * CAP + (j + 1) * 128],
                                                                            in_=xg[:, j, k * 128:(k + 1) * 128], identity=g.idb[:]),
                               reads=[xgtb, g.idbb], writes=[Ptb], signal=(i == n_tr))
                    src = Pv[:, 0:2 * CAP].rearrange("p (k t) -> p k t", k=2)
                    if kp % 2 == 0:
                        op("act", lambda e: e.activation(out=xT[:, 2 * kp:2 * kp + 2, :], in_=src, func=AF.Copy), reads=[Ptb], writes=[xTb])
                    else:
                        op("dve", lambda e: e.tensor_copy(out=xT[:, 2 * kp:2 * kp + 2, :], in_=src), reads=[Ptb], writes=[xTb])

            cur = prep(0) if ne_run > 0 else None
            if cur is not None:
                transposes(cur)
            for ex in range(ne_run):
                st = cur
                nxt = prep(ex + 1) if ex + 1 < ne_run else None
                w1c, w2c, b1, b1b, b2, b2b, xT, xTb = st.w1c, st.w2c, st.b1, st.b1b, st.b2, st.b2b, st.xT, st.xTb
                aT, aTb = r_aT.get()
                for f in range(8):
                    fs = slice((f % 4) * 128, (f % 4 + 1) * 128)
                    wg, wgb_ = w1c[f // 4]
                    wl, wlb_ = w1c[2 + f // 4]
                    Hg, Hgb = g.bank()
                    mm_group(g, Hg[:, 0:CAP], Hgb, [(wg[:, k, fs], xT[:, k, :]) for k in range(8)], reads=[wgb_, xTb])
                    Hl, Hlb = g.bank()
                    mm_group(g, Hl[:, 0:CAP], Hlb, [(wl[:, k, fs], xT[:, k, :]) for k in range(8)], reads=[wlb_, xTb])
                    hg, hgb = r_hg.get()
                    op("dve", lambda e: e.tensor_scalar(out=hg[:], in0=Hg[:, 0:CAP], scalar1=b1[:, f:f + 1], scalar2=7.0, op0=ALU.add, op1=ALU.min),
                       reads=[Hgb, b1b], writes=[hgb])
                    sg, sgb = r_sg.get()
                    op("act", lambda e: e.activation(out=sg[:], in_=hg[:], func=AF.Sigmoid, scale=1.702), reads=[hgb], writes=[sgb])
                    hl, hlb = r_hl.get()
                    op("dve", lambda e: e.tensor_scalar(out=hl[:], in0=Hl[:, 0:CAP], scalar1=b1[:, 8 + f:9 + f], scalar2=7.0, op0=ALU.add, op1=ALU.min),
                       reads=[Hlb, b1b], writes=[hlb])
                    op("dve", lambda e: e.tensor_scalar(out=hl[:], in0=hl[:], scalar1=-7.0, scalar2=1.0, op0=ALU.max, op1=ALU.add),
                       reads=[hlb], writes=[hlb])
                    a1, a1b = r_a1.get()
                    op("pool", lambda e: e.tensor_tensor(out=a1[:], in0=hg[:], in1=sg[:], op=ALU.mult), reads=[hgb, sgb], writes=[a1b])
                    op("dve", lambda e: e.tensor_tensor(out=aT[:, f, :], in0=a1[:], in1=hl[:], op=ALU.mult), reads=[a1b, hlb], writes=[aTb])
                if nxt is not None:
                    transposes(nxt)
                for tt in range(NJ):
                    ysb, ysbb = r_y.get()
                    for half in range(2):
                        w2t, w2b = w2c[half]
                        Y, Yb = g.bank()
                        pairs = [(aT[:, f, tt * 128:(tt + 1) * 128], w2t[:, f, :]) for f in range(8)]
                        pairs.append((onesb[0:1, :], b2[0:1, half * 512:(half + 1) * 512]))
                        mm_group(g, Y[:], Yb, pairs, reads=[aTb, w2b, b2b, cb16])
                        if half == 0:
                            op("act", lambda e: e.activation(out=ysb[:, 0:512], in_=Y[:], func=AF.Copy), reads=[Yb], writes=[ysbb])
                        else:
                            op("dve", lambda e: e.tensor_copy(out=ysb[:, 512:1024], in_=Y[:]), reads=[Yb], writes=[ysbb])
                    r0 = ex * CAP + tt * 128
                    kb.dma("sp", Yg[r0:r0 + 128, :], ysb[:], reads=[ysbb], writes=[ygb])
                cur = nxt
            kb.barrier()

        with ExitStack() as es2:
            al2 = allocator(nc, es2)
            lng = al2("lng2", [128, D], F32)
            lnb = al2("lnb2", [128, D], F32)
            lngb, lnbb = Buf(), Buf()
            kb.dma("sp", lng[:], g.ln2g[0:1, :].broadcast_to([128, D]), writes=[lngb])
            kb.dma("sp", lnb[:], g.ln2b[0:1, :].broadcast_to([128, D]), writes=[lnbb])
            r_h = Ring(al2, "f_h", [128, D], F32, 2)
            r_st = Ring(al2, "f_st", [128, 8], F32, 2)
            r_yk = Ring(al2, "f_yk", [128, 4, D], BF16, 4)
            r_ac = Ring(al2, "f_ac", [128, D], F32, 2)
            for i in range(4):
                op("pool", lambda e, i=i: e.memset(r_yk.t[i][:], 0.0), writes=[r_yk.b[i]])
            g.out_evs = []

            def gathers(T):
                yk, ykb = r_yk.get()
                if ne_run == NE:
                    for k in range(4):
                        kb.idma(yk[:, k, :], Yg[:, :], didx[:, T, k:k + 1], False, NR - 1, reads=[didxb[T], ygb], writes=[ykb])
                return yk, ykb
            pend = [gathers(0), gathers(1), gathers(2)]
            for T in range(NT):
                ht, htb = r_h.get()
                kb.dma("sp", ht[:], g.hs[T * 128:(T + 1) * 128, :], writes=[htb])
                yk, ykb = pend.pop(0)
                if T + 3 < NT:
                    pend.append(gathers(T + 3))
                ac, acb = r_ac.get()
                op("dve", lambda e: e.tensor_scalar(out=ac[:], in0=yk[:, 0, :], scalar1=gk[:, T, 0:1], scalar2=None, op0=ALU.mult),
                   reads=[ykb, gkb[T]], writes=[acb])
                for k in range(1, 4):
                    op("dve", lambda e, k=k: e.scalar_tensor_tensor(out=ac[:], in0=yk[:, k, :], scalar=gk[:, T, k:k + 1], in1=ac[:],
                                                                    op0=ALU.mult, op1=ALU.add), reads=[ykb, gkb[T], acb], writes=[acb])
                op("dve", lambda e: e.scalar_tensor_tensor(out=ac[:], in0=ht[:], scalar=ALPHA, in1=ac[:], op0=ALU.mult, op1=ALU.add),
                   reads=[htb, acb], writes=[acb])
                layer_norm(g, ac, acb, ht, htb, lng, lngb, lnb, lnbb, r_st, last="dve")
                g.out_evs.append(kb.dma("sp", g.out[T * 128:(T + 1) * 128, :], ht[:], reads=[htb]))
            kb.barrier()


def moe_dense(g, stage="full"):
    ne_run = ne_decl(stage)
    nc, kb = g.nc, g.kb
    cst, cstb = g.cst, g.cstb
    op = kb.op
    ident = cst[:, K_ID:K_ID + 128]
    with ExitStack() as es:
        al = allocator(nc, es)
        hT = al("hT", [128, 8, HALF], BF16)
        hTb = [Buf() for _ in range(4)]
        gate = al("gate", [128, NT, NE], F32)
        gateb = [Buf() for _ in range(NT)]
        acc = al("acc", [128, NT, D], F32)
        accb = [Buf() for _ in range(NT)]
        onesb = al("onesb", [1, 128], BF16)
        onesbb = Buf()
        op("dve", lambda e: e.tensor_copy(out=onesb[:], in_=cst[0:1, K_ONE:K_ONE + 128]), reads=[cstb], writes=[onesbb])

        with ExitStack() as es2:
            al2 = allocator(nc, es2)
            wr = al2("wr", [128, 8, NE], F32)
            wrb = Buf()
            kb.dma("sp", wr[:], g.w_r.rearrange("(k p) n -> p k n", p=128), writes=[wrb])
            br = al2("br", [1, NE], F32)
            brb = Buf()
            kb.dma("sp", br[:], g.b_r[0:1, :], writes=[brb])
            r_h = Ring(al2, "r_h", [128, D], F32, 2)
            r_h32 = Ring(al2, "r_h32", [128, 8, 128], F32, 2)
            r_lg = Ring(al2, "r_lg", [128, 3, NE], F32, 2)
            r_sm = Ring(al2, "r_sm", [128, 16], F32, 2)
            for T in range(NT if DBG.get("router", 1) else 0):
                ht, htb = r_h.get()
                kb.dma("sp", ht[:], g.hs[T * 128:(T + 1) * 128, :], writes=[htb])
                h32, h32b = r_h32.get()
                for half in range(2):
                    Pt, Ptb = g.bank()
                    for kk in range(4 if DBG.get("tr", 1) else 0):
                        k = half * 4 + kk
                        op("pe", lambda e, k=k, kk=kk: e.transpose(out=Pt[:, kk * 128:(kk + 1) * 128], in_=ht[:, k * 128:(k + 1) * 128],
                                                                   identity=ident), reads=[htb, cstb], writes=[Ptb], signal=(kk == 3))
                    if DBG.get("r_act", 1):
                        op("act", lambda e: e.activation(out=h32[:, half * 4:(half + 1) * 4, :], in_=Pt[:].rearrange("p (k t) -> p k t", k=4),
                                                         func=AF.Copy), reads=[Ptb], writes=[h32b])
                    if DBG.get("r_dve", 1):
                      op("dve", lambda e: e.tensor_copy(out=hT[:, half * 4:(half + 1) * 4, T * 128:(T + 1) * 128],
                                                      in_=Pt[:].rearrange("p (k t) -> p k t", k=4)), reads=[Ptb], writes=[hTb[T // 4]])
                if not DBG.get("router_mm", 1):
                    if DBG.get("r_pool", 1):
                        op("pool", lambda e: e.memset(gate[:, T, :], 0.0), writes=[gateb[T]])
                    continue
                L, Lb = g.bank()
                pairs = [(h32[:, k, :], wr[:, k, :]) for k in range(8)] + [(cst[0:1, K_ONE:K_ONE + 128], br[0:1, :])]
                mm_group(g, L[:, 0:NE], Lb, pairs, reads=[h32b, wrb, brb, cstb])
                lg, lgb = r_lg.get()
                sm, smb = r_sm.get()
                op("dve", lambda e: e.tensor_copy(out=lg[:, 0, :], in_=L[:, 0:NE]), reads=[Lb], writes=[lgb])
                op("dve", lambda e: e.max(out=sm[:, 0:8], in_=lg[:, 0, :]), reads=[lgb], writes=[smb])
                op("dve", lambda e: e.tensor_scalar(out=lg[:, 1, :], in0=lg[:, 0, :], scalar1=sm[:, 3:4], scalar2=None, op0=ALU.is_ge),
                   reads=[lgb, smb], writes=[lgb])
                op("dve", lambda e: e.tensor_scalar(out=sm[:, 8:9], in0=sm[:, 0:1], scalar1=-1.0, scalar2=None, op0=ALU.mult),
                   reads=[smb], writes=[smb])
                op("act", lambda e: e.activation(out=lg[:, 2, :], in_=lg[:, 0, :], func=AF.Exp, bias=sm[:, 8:9]), reads=[lgb, smb], writes=[lgb])
                op("dve", lambda e: e.scalar_tensor_tensor(out=lg[:, 2, :], in0=lg[:, 2, :], scalar=1.0, in1=lg[:, 1, :], op0=ALU.mult,
                                                           op1=ALU.mult, accum_out=sm[:, 9:10]), reads=[lgb], writes=[lgb, smb])
                op("dve", lambda e: e.reciprocal(out=sm[:, 10:11], in_=sm[:, 9:10]), reads=[smb], writes=[smb])
                op("dve", lambda e: e.tensor_scalar(out=gate[:, T, :], in0=lg[:, 2, :], scalar1=sm[:, 10:11], scalar2=None, op0=ALU.mult),
                   reads=[lgb, smb], writes=[gateb[T]])
            kb.barrier()

        with ExitStack() as es2:
            al2 = allocator(nc, es2)
            wch = Ring(al2, "wch", [128, 8, 512], BF16, 7)
            r_b1 = Ring(al2, "r_b1", [128, 16], F32, 2)
            r_b2 = Ring(al2, "r_b2", [1, D], BF16, 2)
            r_aT = Ring(al2, "r_aT", [128, 8, 512], BF16, 2)
            r_hg = Ring(al2, "r_hg", [128, 512], F32, 2)
            r_sg = Ring(al2, "r_sgm", [128, 512], BF16, 2)
            r_hl = Ring(al2, "r_hl", [128, 512], F32, 2)
            r_a1 = Ring(al2, "r_a1", [128, 512], BF16, 2)
            if ne_run == 0:
                for T in range(NT):
                    op("pool", lambda e: e.memset(acc[:, T, :], 0.0), writes=[accb[T]])
            for ex in range(ne_run):
                ch = []
                for q in [0, 2, 1, 3]:
                    t_, b_ = wch.get()
                    w_dma(g, t_[:], g.w1[ex, :, q * 512:(q + 1) * 512], b_)
                    ch.append((q, t_, b_))
                w1c = {q: (t_, b_) for q, t_, b_ in ch}
                w2c = []
                for half in range(2):
                    t_, b_ = wch.get()
                    w_dma(g, t_[:], g.w2[ex, :, half * 512:(half + 1) * 512], b_)
                    w2c.append((t_, b_))
                b1, b1b = r_b1.get()
                kb.dma("sp", b1[:], g.b1[ex, :, :], writes=[b1b])
                b2, b2b = r_b2.get()
                kb.dma("pool", b2[:], g.b2[ex:ex + 1, :], writes=[b2b])
                for B in range(4):
                    bc = slice(B * 512, (B + 1) * 512)
                    aT, aTb = r_aT.get()
                    for f in range(8):
                        fs = slice((f % 4) * 128, (f % 4 + 1) * 128)
                        wg, wgb_ = w1c[f // 4]
                        wl, wlb_ = w1c[2 + f // 4]
                        Hg, Hgb = g.bank()
                        mm_group(g, Hg[:], Hgb, [(wg[:, k, fs], hT[:, k, bc]) for k in range(8)], reads=[wgb_, hTb[B]])
                        Hl, Hlb = g.bank()
                        mm_group(g, Hl[:], Hlb, [(wl[:, k, fs], hT[:, k, bc]) for k in range(8)], reads=[wlb_, hTb[B]])
                        hg, hgb = r_hg.get()
                        op("dve", lambda e: e.tensor_scalar(out=hg[:], in0=Hg[:], scalar1=b1[:, f:f + 1], scalar2=7.0, op0=ALU.add, op1=ALU.min),
                           reads=[Hgb, b1b], writes=[hgb])
                        sg, sgb = r_sg.get()
                        op("act", lambda e: e.activation(out=sg[:], in_=hg[:], func=AF.Sigmoid, scale=1.702), reads=[hgb], writes=[sgb])
                        hl, hlb = r_hl.get()
                        op("dve", lambda e: e.tensor_scalar(out=hl[:], in0=Hl[:], scalar1=b1[:, 8 + f:9 + f], scalar2=7.0, op0=ALU.add, op1=ALU.min),
                           reads=[Hlb, b1b], writes=[hlb])
                        op("dve", lambda e: e.tensor_scalar(out=hl[:], in0=hl[:], scalar1=-7.0, scalar2=1.0, op0=ALU.max, op1=ALU.add),
                           reads=[hlb], writes=[hlb])
                        a1, a1b = r_a1.get()
                        op("pool", lambda e: e.tensor_tensor(out=a1[:], in0=hg[:], in1=sg[:], op=ALU.mult), reads=[hgb, sgb], writes=[a1b])
                        op("dve", lambda e: e.tensor_tensor(out=aT[:, f, :], in0=a1[:], in1=hl[:], op=ALU.mult), reads=[a1b, hlb], writes=[aTb])
                    for tt in range(4):
                        T = 4 * B + tt
                        for half in range(2):
                            w2t, w2b = w2c[half]
                            Y, Yb = g.bank()
                            pairs = [(aT[:, f, tt * 128:(tt + 1) * 128], w2t[:, f, :]) for f in range(8)]
                            pairs.append((onesb[0:1, :], b2[0:1, half * 512:(half + 1) * 512]))
                            mm_group(g, Y[:], Yb, pairs, reads=[aTb, w2b, b2b, onesbb])
                            dst = acc[:, T, half * 512:(half + 1) * 512]
                            if ex == 0:
                                op("dve", lambda e: e.tensor_scalar(out=dst, in0=Y[:], scalar1=gate[:, T, ex:ex + 1], scalar2=None, op0=ALU.mult),
                                   reads=[Yb, gateb[T]], writes=[accb[T]])
                            else:
                                op("dve", lambda e: e.scalar_tensor_tensor(out=dst, in0=Y[:], scalar=gate[:, T, ex:ex + 1], in1=dst,
                                                                           op0=ALU.mult, op1=ALU.add), reads=[Yb, gateb[T], accb[T]], writes=[accb[T]])
            kb.barrier()

        with ExitStack() as es2:
            al2 = allocator(nc, es2)
            lng = al2("lng2", [128, D], F32)
            lnb = al2("lnb2", [128, D], F32)
            lngb, lnbb = Buf(), Buf()
            kb.dma("sp", lng[:], g.ln2g[0:1, :].broadcast_to([128, D]), writes=[lngb])
            kb.dma("sp", lnb[:], g.ln2b[0:1, :].broadcast_to([128, D]), writes=[lnbb])
            r_h = Ring(al2, "f_h", [128, D], F32, 2)
            r_st = Ring(al2, "f_st", [128, 8], F32, 2)
            g.out_evs = []
            for T in range(NT):
                ht, htb = r_h.get()
                kb.dma("sp", ht[:], g.hs[T * 128:(T + 1) * 128, :], writes=[htb])
                op("dve", lambda e: e.scalar_tensor_tensor(out=acc[:, T, :], in0=ht[:], scalar=ALPHA, in1=acc[:, T, :], op0=ALU.mult, op1=ALU.add),
                   reads=[htb, accb[T]], writes=[accb[T]])
                layer_norm(g, acc[:, T, :], accb[T], ht, htb, lng, lngb, lnb, lnbb, r_st)
                g.out_evs.append(kb.dma("sp", g.out[T * 128:(T + 1) * 128, :], ht[:], reads=[htb]))
            kb.barrier()


def shard_inputs(inputs, stage):
    x = np.asarray(inputs["x"], np.float32)
    w_in = np.asarray(inputs["w_in"], np.float32)[0]
    w_in_sw = w_in.copy()
    w_in_sw[:, C_FF:C_FF + 512] = w_in[:, C_FB:C_FB + 512]
    w_in_sw[:, C_FB:C_FB + 512] = w_in[:, C_FF:C_FF + 512]
    conv_w = np.asarray(inputs["conv_w"], np.float32)[0]
    lbl = np.asarray(inputs["hgrn_lb_logits"], np.float32)
    consts = make_consts()
    maps = []
    for c in range(8):
        b, hf = c // 2, c % 2
        xl = x[b] if hf == 0 else x[b][::-1]
        cw = conv_w if hf == 0 else conv_w[::-1]
        ll = lbl if hf == 0 else lbl[::-1]
        m = {
            "xT": np.ascontiguousarray(xl.T),
            "xo": np.ascontiguousarray(xl[:HALF]),
            "w_in": w_in if hf == 0 else w_in_sw,
            "convw_t": np.ascontiguousarray(cw.T),
            "lbl": np.ascontiguousarray(ll.reshape(1, 2048)),
            "lbl_t": np.ascontiguousarray(ll.reshape(4, 512).T),
            "normw_t": np.ascontiguousarray(np.asarray(inputs["hgrn_norm_w"], np.float32)[0].reshape(4, 128).T),
            "p_a": np.asarray(inputs["p_a"], np.float32)[0],
            "p_b": np.asarray(inputs["p_b"], np.float32)[0],
            "w_o": np.asarray(inputs["w_o"], np.float32)[0],
            "ln1_g": np.asarray(inputs["ln1_g"], np.float32),
            "ln1_b": np.asarray(inputs["ln1_b"], np.float32),
            "consts": consts,
        }
        if stage in FULLS:
            b1 = np.asarray(inputs["b1"], np.float32)[0]
            m.update({
                "w_router": np.asarray(inputs["w_router"], np.float32)[0],
                "b_router": np.asarray(inputs["b_router"], np.float32),
                "ln2_g": np.asarray(inputs["ln2_g"], np.float32),
                "ln2_b": np.asarray(inputs["ln2_b"], np.float32),
            })
            if "router" not in stage:
                nd = ne_decl(stage)
                m.update({
                    "w1": np.asarray(inputs["w1"], np.float32)[0][:nd],
                    "b1t": np.ascontiguousarray(b1.reshape(NE, 16, 128).transpose(0, 2, 1))[:nd],
                    "w2": np.asarray(inputs["w2"], np.float32)[0][:nd],
                    "b2": np.asarray(inputs["b2"], np.float32)[0][:nd],
                })
        if stage.startswith("m_"):
            hl = inputs["h_in"][b] if hf == 0 else inputs["h_in"][b][::-1]
            m["h_in"] = np.ascontiguousarray(hl[:HALF])
        maps.append(m)
    return maps


def run(inputs, stage="full", key="out"):
    nc = build_nc(stage)
    maps = shard_inputs(inputs, stage)
    res = run_bass_kernel_spmd(nc, maps, core_ids=list(range(8)))
    out = np.zeros((4, SEQ, D), np.float32)
    for c in range(8):
        b, hf = c // 2, c % 2
        y = np.asarray(res.results[c][key], np.float32)
        if hf == 0:
            out[b, :HALF] = y
        else:
            out[b, HALF:] = y[::-1]
    return out


def kernel(**inputs):
    return run(inputs, "full", "out")
```

```python
import numpy as np
from contextlib import ExitStack
import concourse.bass as bass
import concourse.mybir as mybir
from concourse.bass_utils import run_bass_kernel_spmd

F32 = mybir.dt.float32
BF16 = mybir.dt.bfloat16
AF = mybir.ActivationFunctionType
ALU = mybir.AluOpType

D = 1024
SEQ = 4096
HALF = 2048
NT = 16
NE = 32
DFF = 1024
ALPHA = 2.0 ** 0.25
LN_EPS = 1e-5
RMS_EPS = 1e-6
NDMA = 24
DBG = {}
FULLS = ("full", "t_router", "m_router", "m_full") + tuple("%s_e%d" % (a, n) for a in "tm" for n in (1, 2, 4, 8, 16))

C_B, C_C, C_H, C_Q, C_FF, C_FB, C_I, C_G, C_GA, C_GB = 0, 512, 1024, 1536, 2048, 2560, 3072, 3584, 4096, 5120

K_MKF, K_MKB, K_M3F, K_M3B, K_ID, K_ONE = 0, 260, 520, 648, 776, 904
K_EC, K_LT = 1032, 1064
NCONST = 1192
CAP = 384


def make_consts():
    c = np.zeros((128, NCONST), np.float32)
    s = np.arange(128)[:, None]
    t = np.arange(128)[None, :]
    same = (s // 32) == (t // 32)
    cum_f = same & (s <= t)
    cum_b = same & (s >= t)
    ref_f = same & ((s % 32) <= 15)
    ref_b = same & ((s % 32) >= 16)
    c[:, K_MKF:K_MKF + 128] = cum_f.astype(np.float32) - ref_f.astype(np.float32)
    c[:, K_MKF + 128:K_MKF + 256] = cum_f
    c[:, K_MKB:K_MKB + 128] = cum_b.astype(np.float32) - ref_b.astype(np.float32)
    c[:, K_MKB + 128:K_MKB + 256] = cum_b
    for k in range(4):
        c[:, K_MKF + 256 + k] = (np.arange(128) // 32 == k)
        c[:, K_MKB + 256 + k] = (np.arange(128) // 32 == 3 - k)
    c[:, K_M3F:K_M3F + 128] = same & (s > t)
    c[:, K_M3B:K_M3B + 128] = same & (s < t)
    c[:, K_ID:K_ID + 128] = np.eye(128)
    c[:, K_ONE:K_ONE + 128] = 1.0
    c[:, K_EC:K_EC + NE] = (np.arange(NE) * CAP)[None, :]
    c[:, K_LT:K_LT + 128] = (s < t)
    return c


def ne_decl(stage):
    if "_e" in stage:
        return int(stage.split("_e")[1])
    return 0 if "router" in stage else NE


class Buf:
    __slots__ = ("w", "r", "excl")

    def __init__(self, excl=False):
        self.w = None
        self.r = {}
        self.excl = excl


class KB:
    def __init__(self, nc):
        self.nc = nc
        self.E = dict(pe=nc.tensor, act=nc.scalar, dve=nc.vector, pool=nc.gpsimd, sp=nc.sync)
        self.sem = {k: nc.alloc_semaphore("s_" + k) for k in self.E}
        self.cnt = {k: 0 for k in self.E}
        self.seen = {k: {} for k in self.E}
        self.dsem = [[nc.alloc_semaphore("d%d" % i), 0] for i in range(2 * NDMA)]
        self.dnext = {"sp": 0, "pool": 0}
        self.nwait = 0

    def _deps(self, reads, writes, e=None):
        deps = {}

        def add(k, v):
            if deps.get(k, 0) < v:
                deps[k] = v
        for b in reads:
            if b.w is not None:
                add(*b.w)
            if b.excl:
                for k, v in b.r.items():
                    if k != e:
                        add(k, v)
        for b in writes:
            if b.w is not None:
                add(*b.w)
            for k, v in b.r.items():
                add(k, v)
        return deps

    def _wait(self, e, deps):
        for k, v in deps.items():
            if k == e and e in ("pe", "sp"):
                continue
            if self.seen[e].get(k, 0) >= v:
                continue
            self.seen[e][k] = v
            sem = self.sem[k] if isinstance(k, str) else self.dsem[k][0]
            self.E[e].wait_ge(sem, v)
            self.nwait += 1

    def _mark(self, ev, reads, writes):
        k, v = ev
        for b in reads:
            if b.r.get(k, 0) < v:
                b.r[k] = v
        for b in writes:
            b.w = ev
            b.r = {}

    def op(self, e, fn, reads=(), writes=(), signal=True):
        self._wait(e, self._deps(reads, writes, e))
        ins = fn(self.E[e])
        if signal:
            self.cnt[e] += 1
            ins.then_inc(self.sem[e], 1)
            ev = (e, self.cnt[e])
        else:
            ev = (e, self.cnt[e] + 1)
        self._mark(ev, reads, writes)
        return ev

    def dma(self, q, out, in_, reads=(), writes=(), **kw):
        deps = self._deps(reads, writes)
        idx = self.dnext[q] + (NDMA if q == "pool" else 0)
        self.dnext[q] = (self.dnext[q] + 1) % NDMA
        sem, val = self.dsem[idx]
        if val > 0 and deps.get(idx, 0) < val:
            deps[idx] = val
        self._wait(q, deps)
        ins = self.E[q].dma_start(out=out, in_=in_, **kw)
        val += 16
        self.dsem[idx][1] = val
        ins.then_inc(sem, 16)
        ev = (idx, val)
        self._mark(ev, reads, writes)
        return ev

    def idma(self, out, in_, idx_ap, scatter, bound, reads=(), writes=()):
        q = "pool"
        deps = self._deps(reads, writes)
        idx = self.dnext[q] + NDMA
        self.dnext[q] = (self.dnext[q] + 1) % NDMA
        sem, val = self.dsem[idx]
        if val > 0 and deps.get(idx, 0) < val:
            deps[idx] = val
        self._wait(q, deps)
        off = bass.IndirectOffsetOnAxis(ap=idx_ap, axis=0)
        if not hasattr(self, "_bound_regs"):
            self._bound_regs = {}
        if bound not in self._bound_regs:
            self._bound_regs[bound] = self.E[q].to_reg(bound)
        bound = self._bound_regs[bound]
        if scatter:
            ins = self.E[q].indirect_dma_start(out=out, out_offset=off, in_=in_, in_offset=None, bounds_check=bound, oob_is_err=False)
        else:
            ins = self.E[q].indirect_dma_start(out=out, out_offset=None, in_=in_, in_offset=off, bounds_check=bound, oob_is_err=False)
        val += 16
        self.dsem[idx][1] = val
        ins.then_inc(sem, 16)
        ev = (idx, val)
        self._mark(ev, reads, writes)
        return ev

    def barrier(self):
        for e in self.E:
            deps = {k: v for k, v in self.cnt.items() if v > 0}
            for i, (sem, val) in enumerate(self.dsem):
                if val > 0:
                    deps[i] = val
            self._wait(e, deps)

    def wait_ev(self, e, ev):
        self._wait(e, {ev[0]: ev[1]})


class Ctx:
    pass


def build_nc(stage="full"):
    nc = bass.Bass("TRN2", target_bir_lowering=False)
    kb = KB(nc)
    g = Ctx()
    g.nc, g.kb = nc, kb

    def din(name, shape):
        return nc.dram_tensor(name, list(shape), F32, kind="ExternalInput").ap()

    g.xT = din("xT", [D, SEQ])
    g.xo = din("xo", [HALF, D])
    g.w_in = din("w_in", [D, 6144])
    g.convw = din("convw_t", [512, 3])
    g.lbl = din("lbl", [1, 2048])
    g.lblt = din("lbl_t", [512, 4])
    g.normw = din("normw_t", [128, 4])
    g.p_a = din("p_a", [512, D])
    g.p_b = din("p_b", [512, D])
    g.w_o = din("w_o", [D, D])
    g.ln1g = din("ln1_g", [1, D])
    g.ln1b = din("ln1_b", [1, D])
    g.consts = din("consts", [128, NCONST])
    if stage in FULLS:
        g.w_r = din("w_router", [D, NE])
        g.b_r = din("b_router", [1, NE])
        if "router" not in stage:
            nd = ne_decl(stage)
            g.w1 = din("w1", [nd, D, 2 * DFF])
            g.b1 = din("b1t", [nd, 128, 16])
            g.w2 = din("w2", [nd, DFF, D])
            g.b2 = din("b2", [nd, D])
        g.ln2g = din("ln2_g", [1, D])
        g.ln2b = din("ln2_b", [1, D])
    g.out = nc.dram_tensor("out", [HALF, D], F32, kind="ExternalOutput").ap()
    if stage.startswith("m_"):
        g.hs = din("h_in", [HALF, D])
    else:
        g.hs = nc.dram_tensor("h_scratch", [HALF, D], F32, kind="Internal").ap()

    g.Xg = nc.dram_tensor("xg_scratch", [NE * CAP, D], BF16, kind="Internal").ap()
    g.xgb = Buf()

    g.banks = [(nc.alloc_psum_tensor("bank%d" % i, [128, 512], F32), Buf(excl=True)) for i in range(8)]
    g.bnext = 0

    def bank():
        b = g.banks[g.bnext]
        g.bnext = (g.bnext + 1) % 8
        return b
    g.bank = bank
    g.pools = {"P": [0, 1], "E": [2, 3, 4, 5], "K": [6, 7]}
    g.pnext = {"P": 0, "E": 0, "K": 0}

    def bank_from(name):
        lst = g.pools[name]
        b = g.banks[lst[g.pnext[name] % len(lst)]]
        g.pnext[name] += 1
        return b
    g.bank_from = bank_from

    g.cst = nc.alloc_sbuf_tensor("cst", [128, NCONST], F32)
    g.cstb = Buf()
    kb.dma("sp", g.cst[:], g.consts[:, :], writes=[g.cstb])
    g.idb = nc.alloc_sbuf_tensor("idb", [128, 128], BF16)
    g.idbb = Buf()
    kb.op("dve", lambda e: e.tensor_copy(out=g.idb[:], in_=g.cst[:, K_ID:K_ID + 128]), reads=[g.cstb], writes=[g.idbb])

    if not stage.startswith("m_"):
        with ExitStack() as es0:
            mixer(g, es0, stage)
    if stage == "t_hs":
        with ExitStack() as es:
            al = allocator(nc, es)
            rr = Ring(al, "thsr", [128, D], F32, 2)
            g.out_evs = []
            for T in range(NT):
                t_, b_ = rr.get()
                kb.dma("sp", t_[:], g.hs[T * 128:(T + 1) * 128, :], writes=[b_])
                g.out_evs.append(kb.dma("sp", g.out[T * 128:(T + 1) * 128, :], t_[:], reads=[b_]))
    elif not stage.startswith("mixer"):
        moe(g, stage)
    for ev in g.out_evs + getattr(g, "dbg_evs", []) + getattr(g, "dbg_evs2", []):
        kb.wait_ev("sp", ev)
    return nc


class Ring:
    def __init__(self, al, name, shape, dtype, n):
        self.t = [al("%s%d" % (name, i), shape, dtype) for i in range(n)]
        self.b = [Buf() for _ in range(n)]
        self.i = 0
        self.n = n

    def get(self):
        r = (self.t[self.i], self.b[self.i])
        self.i = (self.i + 1) % self.n
        return r


def mm_group(g, out_ap, outbuf, pairs, reads, f32=False, start=True, stop=True):
    kb = g.kb
    n = len(pairs)
    ev = None
    for i, (l, r) in enumerate(pairs):
        st = start and i == 0
        sp = stop and i == n - 1
        ev = kb.op("pe", lambda e, l=l, r=r, st=st, sp=sp: e.matmul(out_ap, lhsT=l, rhs=r, start=st, stop=sp),
                   reads=reads, writes=[outbuf], signal=(i == n - 1))
    return ev


def allocator(nc, es):
    def al(name, shape, dtype):
        return es.enter_context(nc.sbuf_tensor(name, list(shape), dtype))
    return al


def xT_dma(g, dst, t0, n, buf):
    return g.kb.dma("pool", dst, g.xT[:, t0:t0 + n].rearrange("(k p) t -> p k t", p=128), writes=[buf])


def w_dma(g, dst, src, buf):
    return g.kb.dma("pool", dst, src.rearrange("(k p) n -> p k n", p=128), writes=[buf])


def mixer(g, es0, stage):
    nc, kb = g.nc, g.kb
    cst, cstb = g.cst, g.cstb
    op = kb.op
    al0 = allocator(nc, es0)

    xTo = al0("xTo", [128, 8, HALF + 8], BF16)
    xTob = [Buf() for _ in range(5)]
    for B in range(4):
        xT_dma(g, xTo[:, :, B * 512:(B + 1) * 512], B * 512, 512, xTob[B])
    xT_dma(g, xTo[:, :, HALF:HALF + 8], HALF, 8, xTob[4])
    ybT = al0("ybT", [128, 4, HALF], BF16)
    ybb = [[Buf() for _ in range(4)] for _ in range(4)]
    yaT = al0("yaT", [128, 4, HALF], BF16)
    yab = [Buf() for _ in range(4)]

    mk = [cst[:, K_MKF:K_MKF + 260], cst[:, K_MKB:K_MKB + 260]]
    m3 = [cst[:, K_M3F:K_M3F + 128], cst[:, K_M3B:K_M3B + 128]]
    ones = cst[:, K_ONE:K_ONE + 128]

    def cmask(dr, k):
        base = K_MKF if dr == 0 else K_MKB
        return cst[:, base + 256 + k:base + 257 + k]

    with ExitStack() as es:
        al = allocator(nc, es)
        lraw = al("lraw", [128, 4, 4], F32)
        lrawb = Buf()
        kb.dma("sp", lraw[:], g.lblt.rearrange("(h p) c -> p h c", p=128), writes=[lrawb])
        omlc = al("omlc", [128, 2, 4], F32)
        omlcb = Buf()
        for a in range(2):
            op("dve", lambda e, a=a: e.tensor_tensor(out=omlc[:, a, :], in0=lraw[:, :, 2 * a + 1], in1=lraw[:, :, 2 * a],
                                                     op=ALU.subtract), reads=[lrawb], writes=[omlcb])
        op("act", lambda e: e.activation(out=omlc[:], in_=omlc[:], func=AF.Sigmoid), reads=[omlcb], writes=[omlcb])
        omlt = al("omlt", [128, 2, 512], F32)
        omltb = Buf()
        with ExitStack() as es2:
            lbig = allocator(nc, es2)("lbig", [128, 2048], F32)
            lbigb = Buf()
            kb.dma("sp", lbig[:], g.lbl[0:1, :].broadcast_to([128, 2048]), writes=[lbigb])
            for a in range(2):
                op("dve", lambda e, a=a: e.tensor_tensor(out=omlt[:, a, :], in0=lbig[:, a * 1024 + 512:a * 1024 + 1024],
                                                         in1=lbig[:, a * 1024:a * 1024 + 512], op=ALU.subtract),
                   reads=[lbigb], writes=[omltb])
            kb.barrier()
        op("act", lambda e: e.activation(out=omlt[:], in_=omlt[:], func=AF.Sigmoid), reads=[omltb], writes=[omltb])
        nrm = al("nrm", [128, 4], F32)
        nrmb = Buf()
        kb.dma("sp", nrm[:], g.normw[:, :], writes=[nrmb])

        w_tm = al("w_tm", [128, 8, 384], BF16)
        w_cm = al("w_cm", [128, 8, 512], BF16)
        wtmb, wcmb = Buf(), Buf()
        names = ["qt", "Qh", "kt"]
        big = {(n, dr): al("%s%d" % (n, dr), [128, HALF], BF16) for n in names for dr in range(2)}
        bigb = {(n, dr): [Buf() for _ in range(NT)] for n in names for dr in range(2)}
        V = al("V", [128, NT, 128], BF16)
        Vb = [Buf() for _ in range(NT)]
        sgl = al("sgl", [128, HALF], BF16)
        sglb = [Buf() for _ in range(4)]
        kv = [al("kv%d" % i, [128, 128, 64], BF16) for i in range(2)]
        SCM = 8
        decr = al("decr", [128, SCM, 64], F32)
        decrb = Buf()
        kvb = [Buf() for _ in range(2)]
        dec = [al("dec%d" % i, [128, 64], F32) for i in range(2)]
        decb = [Buf() for _ in range(2)]
        bound = al("bound", [128, 128], F32)
        boundb = Buf()
        bnd16 = al("bnd16", [128, 128], BF16)
        bnd16b = Buf()
        xoth = Ring(al, "xoth", [128, 8, 512], BF16, 2)

        r_sg = Ring(al, "r_sg", [128, 256], F32, 2)
        r_omf = Ring(al, "r_omf", [128, 256], F32, 5)
        r_lf = Ring(al, "r_lf", [128, 256], F32, 2)
        r_ex3 = Ring(al, "r_ex3", [128, 256], F32, 2)
        r_ex12 = Ring(al, "r_ex12", [128, 2, 256], F32, 2)
        r_exm = Ring(al, "r_exm", [128, 2, 128], F32, 2)
        r_khm = Ring(al, "r_khm", [128, 2, 4, 128], BF16, 2)
        r_vo = Ring(al, "r_vo", [128, 128], BF16, 7)
        r_qT = Ring(al, "r_qT", [128, 512], BF16, 3)
        r_sgT = Ring(al, "r_sgT", [128, 2, 512], BF16, 3)
        r_scm = Ring(al, "r_scm", [128, 256], BF16, 2)
        r_f512 = Ring(al, "r_f512", [128, 512], F32, 2)

        def run_pipeline(items, stages):
            ns = len(stages)
            cs = []
            for it in items:
                c = Ctx()
                c.T = it
                cs.append(c)
            if DBG.get("noskew", 0):
                for c in cs:
                    for st_ in stages:
                        st_(c)
                return
            for step in range(len(cs) + ns - 1):
                for si in range(ns):
                    i = step - si
                    if 0 <= i < len(cs):
                        stages[si](cs[i])

        def scan_all(x, fold):
            if fold:
                op("dve", lambda e: e.scalar_tensor_tensor(out=kv[x][:, :, 0], in0=bound[:], scalar=dec[x][:, 0:1], in1=kv[x][:, :, 0],
                                                           op0=ALU.mult, op1=ALU.add), reads=[boundb, decb[x], kvb[x]], writes=[kvb[x]])
            op("act", lambda e: e.activation(out=decr[:], in_=dec[x][:].unsqueeze(1).broadcast_to([128, SCM, 64]), func=AF.Copy),
               reads=[decb[x]], writes=[decrb])
            op("pool", lambda e: e.memset(decr[:, :, 0:1], 0.0), reads=[decrb], writes=[decrb])
            for eg in range(128 // SCM):
                view = kv[x][:, eg * SCM:(eg + 1) * SCM, :].rearrange("p e c -> p (e c)")
                op("dve", lambda e, view=view: e.tensor_tensor_scan(out=view, data0=decr[:].rearrange("p m c -> p (m c)"), data1=view,
                                                                    initial=0.0, op0=ALU.mult, op1=ALU.add),
                   reads=[decrb], writes=[kvb[x]])

        def load_head(hh):
            for i, c0 in enumerate([C_FF, C_FB, C_I]):
                w_dma(g, w_tm[:, :, i * 128:(i + 1) * 128], g.w_in[:, c0 + hh * 128:c0 + (hh + 1) * 128], wtmb)
            for i, c0 in enumerate([C_Q, C_FF, C_FB, C_G]):
                w_dma(g, w_cm[:, :, i * 128:(i + 1) * 128], g.w_in[:, c0 + hh * 128:c0 + (hh + 1) * 128], wcmb)
            pre = xoth.get()
            xT_dma(g, pre[0][:], HALF, 512, pre[1])
            return pre

        xpre = load_head(0)
        for h in range(4):
            hs = slice(h * 128, (h + 1) * 128)

            oth = {}

            def a0(c):
                T = c.T
                Bo, tt = (T - 16) // 4, (T - 16) % 4
                if tt == 0:
                    if Bo == 0:
                        oth[Bo] = xpre
                    else:
                        oth[Bo] = xoth.get()
                        xT_dma(g, oth[Bo][0][:], HALF + Bo * 512, 512, oth[Bo][1])
                xt, xb = oth[Bo]
                c.c0 = 124 - 4 * T
                c.P, c.Pb = g.bank_from("P")
                mm_group(g, c.P[:, 0:256], c.Pb, [(xt[:, k, tt * 128:(tt + 1) * 128], w_tm[:, k, 128:384]) for k in range(8)],
                         reads=[xb, wtmb])

            def a1(c):
                c.sg, c.sgb = r_sg.get()
                op("act", lambda e: e.activation(out=c.sg[:, 0:128], in_=c.P[:, 0:128], func=AF.Exp), reads=[c.Pb], writes=[c.sgb])
                op("act", lambda e: e.activation(out=c.sg[:, 0:128], in_=c.sg[:, 0:128], func=AF.Ln, bias=1.0), reads=[c.sgb], writes=[c.sgb])
                op("act", lambda e: e.activation(out=c.sg[:, 0:128], in_=c.sg[:, 0:128], func=AF.Exp, scale=-1.0), reads=[c.sgb], writes=[c.sgb])
                c.vo, c.vob = r_vo.get()
                op("act", lambda e: e.activation(out=c.vo[:], in_=c.P[:, 128:256], func=AF.Copy), reads=[c.Pb], writes=[c.vob])

            def a2(c):
                c.omf, c.omfb = r_omf.get()
                op("dve", lambda e: e.tensor_tensor(out=c.omf[:, 0:128], in0=c.sg[:, 0:128], in1=omlt[:, 1, hs], op=ALU.mult),
                   reads=[c.sgb, omltb], writes=[c.omfb])

            def a3(c):
                c.lf, c.lfb = r_lf.get()
                op("act", lambda e: e.activation(out=c.lf[:, 0:128], in_=c.omf[:, 0:128], func=AF.Ln, scale=-1.0, bias=1.0),
                   reads=[c.omfb], writes=[c.lfb])

            def a4(c):
                c.Eb, c.Ebb = g.bank_from("E")
                mm_group(g, c.Eb[:, 0:128], c.Ebb, [(m3[1], c.lf[:, 0:128])], reads=[cstb, c.lfb])
                mm_group(g, c.Eb[:, 128:132], c.Ebb, [(c.lf[:, 0:128], mk[1][:, 256:260])], reads=[cstb, c.lfb])

            def a5(c):
                c.ex3, c.ex3b = r_ex3.get()
                op("act", lambda e: e.activation(out=c.ex3[:, 0:128], in_=c.Eb[:, 0:128], func=AF.Exp), reads=[c.Ebb], writes=[c.ex3b])
                op("act", lambda e: e.activation(out=dec[1][:, c.c0:c.c0 + 4], in_=c.Eb[:, 128:132], func=AF.Exp),
                   reads=[c.Ebb], writes=[decb[1]])

            def a6(c):
                c.khm, c.khmb = r_khm.get()
                for k in range(4):
                    op("dve", lambda e, k=k: e.scalar_tensor_tensor(out=c.khm[:, 0, k, :], in0=c.ex3[:, 0:128], scalar=cmask(1, k),
                                                                    in1=c.omf[:, 0:128], op0=ALU.mult, op1=ALU.mult),
                       reads=[c.ex3b, c.omfb, cstb], writes=[c.khmb])

            def a7(c):
                c.Kp, c.Kpb = g.bank_from("K")
                for k in range(4):
                    mm_group(g, c.Kp[:, k * 128:(k + 1) * 128], c.Kpb, [(c.khm[:, 0, k, :], c.vo[:])], reads=[c.khmb, c.vob])
                if c.T % 2 == 0:
                    op("act", lambda e: e.activation(out=kv[1][:, :, c.c0:c.c0 + 4].rearrange("p e c -> p c e"),
                                                     in_=c.Kp[:].rearrange("p (c e) -> p c e", e=128),
                                                     func=AF.Copy), reads=[c.Kpb], writes=[kvb[1]])
                else:
                    op("dve", lambda e: e.tensor_copy(out=kv[1][:, :, c.c0:c.c0 + 4].rearrange("p e c -> p c e"),
                                                      in_=c.Kp[:].rearrange("p (c e) -> p c e", e=128)), reads=[c.Kpb], writes=[kvb[1]])

            run_pipeline(list(range(16, 32)), [a0, a1, a2, a3, a4, a5, a6, a7])
            scan_all(1, fold=False)
            op("act", lambda e: e.activation(out=bound[:], in_=kv[1][:, :, 63], func=AF.Copy), reads=[kvb[1]], writes=[boundb])
            op("act", lambda e: e.activation(out=bnd16[:], in_=kv[1][:, :, 63], func=AF.Copy), reads=[kvb[1]], writes=[bnd16b])

            blk = {}

            def b0(c):
                T = c.T
                B, tt = T // 4, T % 4
                if tt == 0:
                    qT, qTb = r_qT.get()
                    sgT, sgTb = r_sgT.get()
                    for cb in [0, 3, 1, 2]:
                        Pc, Pcb = g.bank_from("K")
                        mm_group(g, Pc[:], Pcb, [(w_cm[:, k, cb * 128:(cb + 1) * 128], xTo[:, k, B * 512:(B + 1) * 512]) for k in range(8)],
                                 reads=[wcmb, xTob[B]])
                        if cb == 0:
                            op("act", lambda e: e.activation(out=qT[:], in_=Pc[:], func=AF.Silu), reads=[Pcb], writes=[qTb])
                        elif cb == 3:
                            op("act", lambda e: e.activation(out=sgl[:, B * 512:(B + 1) * 512], in_=Pc[:], func=AF.Silu),
                               reads=[Pcb], writes=[sglb[B]])
                        else:
                            op("act", lambda e, cb=cb: e.activation(out=sgT[:, cb - 1, :], in_=Pc[:], func=AF.Sigmoid, scale=-1.0),
                               reads=[Pcb], writes=[sgTb])
                    blk[B] = (qT, qTb, sgT, sgTb)
                c.tc = slice(T * 128, (T + 1) * 128)
                c.tl = slice(tt * 128, (tt + 1) * 128)
                c.qT, c.qTb, c.sgT, c.sgTb = blk[B]
                c.P, c.Pb = g.bank_from("P")
                mm_group(g, c.P[:, 0:384], c.Pb, [(xTo[:, k, c.tc], w_tm[:, k, 0:384]) for k in range(8)], reads=[xTob[B], wtmb])

            def b1(c):
                c.sg, c.sgb = r_sg.get()
                op("act", lambda e: e.activation(out=c.sg[:], in_=c.P[:, 0:256], func=AF.Exp), reads=[c.Pb], writes=[c.sgb])
                op("act", lambda e: e.activation(out=c.sg[:], in_=c.sg[:], func=AF.Ln, bias=1.0), reads=[c.sgb], writes=[c.sgb])
                op("act", lambda e: e.activation(out=c.sg[:], in_=c.sg[:], func=AF.Exp, scale=-1.0), reads=[c.sgb], writes=[c.sgb])
                op("act", lambda e: e.activation(out=V[:, c.T, :], in_=c.P[:, 256:384], func=AF.Copy), reads=[c.Pb], writes=[Vb[c.T]])

            def b2(c):
                c.omf, c.omfb = r_omf.get()
                op("dve", lambda e: e.tensor_tensor(out=c.omf[:].rearrange("p (a d) -> p a d", a=2),
                                                    in0=c.sg[:].rearrange("p (a d) -> p a d", a=2),
                                                    in1=omlt[:, :, hs], op=ALU.mult),
                   reads=[c.sgb, omltb], writes=[c.omfb])

            def b3(c):
                c.lf, c.lfb = r_lf.get()
                op("act", lambda e: e.activation(out=c.lf[:], in_=c.omf[:], func=AF.Ln, scale=-1.0, bias=1.0), reads=[c.omfb], writes=[c.lfb])

            def b4(c):
                c.E, c.Eb_ = g.bank_from("E")
                c.E3, c.E3b = g.bank_from("E")
                for dr in range(2):
                    ds = slice(dr * 128, (dr + 1) * 128)
                    mm_group(g, c.E[:, dr * 256:(dr + 1) * 256], c.Eb_, [(c.lf[:, ds], mk[dr][:, 0:256])], reads=[cstb, c.lfb])
                    mm_group(g, c.E3[:, ds], c.E3b, [(m3[dr], c.lf[:, ds])], reads=[cstb, c.lfb])
                    mm_group(g, c.E3[:, 256 + 4 * dr:260 + 4 * dr], c.E3b, [(c.lf[:, ds], mk[dr][:, 256:260])], reads=[cstb, c.lfb])

            def b5(c):
                T = c.T
                c.ex3, c.ex3b = r_ex3.get()
                op("act", lambda e: e.activation(out=c.ex3[:], in_=c.E3[:, 0:256], func=AF.Exp), reads=[c.E3b], writes=[c.ex3b])
                c.ex12, c.ex12b = r_ex12.get()
                op("act", lambda e: e.activation(out=c.ex12[:].rearrange("p a t -> p (a t)"), in_=c.E[:, 0:512], func=AF.Exp),
                   reads=[c.Eb_], writes=[c.ex12b])
                c.exm, c.exmb = r_exm.get()
                op("act", lambda e: e.activation(out=c.exm[:], in_=c.E[:, 0:512].rearrange("p (a t) -> p a t", a=2)[:, :, 0:128],
                                                 func=AF.Exp, scale=-1.0), reads=[c.Eb_], writes=[c.exmb])
                for dr in range(2):
                    c0 = 4 * T if dr == 0 else 60 - 4 * T
                    op("act", lambda e, dr=dr, c0=c0: e.activation(out=dec[dr][:, c0:c0 + 4], in_=c.E3[:, 256 + 4 * dr:260 + 4 * dr], func=AF.Exp),
                       reads=[c.E3b], writes=[decb[dr]])

            def b6(c):
                T = c.T
                c.khm, c.khmb = r_khm.get()
                for dr in range(2):
                    ds = slice(dr * 128, (dr + 1) * 128)
                    op("pool", lambda e, dr=dr: e.tensor_tensor(out=big[("qt", dr)][:, c.tc], in0=c.qT[:, c.tl], in1=c.ex12[:, dr, 0:128], op=ALU.mult),
                       reads=[c.qTb, c.ex12b], writes=[bigb[("qt", dr)][T]])
                    op("pool", lambda e, dr=dr: e.tensor_tensor(out=big[("Qh", dr)][:, c.tc], in0=c.qT[:, c.tl], in1=c.ex12[:, dr, 128:256], op=ALU.mult),
                       reads=[c.qTb, c.ex12b], writes=[bigb[("Qh", dr)][T]])
                    op("dve", lambda e, dr=dr: e.scalar_tensor_tensor(out=big[("kt", dr)][:, c.tc], in0=c.sgT[:, dr, c.tl],
                                                                      scalar=omlc[:, dr, h:h + 1], in1=c.exm[:, dr, :], op0=ALU.mult, op1=ALU.mult),
                       reads=[c.sgTb, omlcb, c.exmb], writes=[bigb[("kt", dr)][T]])
                    for k in range(4):
                        op("dve", lambda e, k=k, dr=dr, ds=ds: e.scalar_tensor_tensor(out=c.khm[:, dr, k, :], in0=c.ex3[:, ds], scalar=cmask(dr, k),
                                                                                      in1=c.omf[:, ds], op0=ALU.mult, op1=ALU.mult),
                           reads=[c.ex3b, c.omfb, cstb], writes=[c.khmb])

            def b7(c):
                T = c.T
                for dr in range(2):
                    Kp, Kpb = g.bank_from("K")
                    for k in range(4):
                        mm_group(g, Kp[:, k * 128:(k + 1) * 128], Kpb, [(c.khm[:, dr, k, :], V[:, c.T, :])], reads=[c.khmb, Vb[c.T]])
                    c0 = 4 * T if dr == 0 else 60 - 4 * T
                    if dr == 0:
                        op("act", lambda e, c0=c0, dr=dr, Kp=Kp: e.activation(out=kv[dr][:, :, c0:c0 + 4].rearrange("p e c -> p c e"),
                                                                              in_=Kp[:].rearrange("p (c e) -> p c e", e=128),
                                                                              func=AF.Copy), reads=[Kpb, boundb, bnd16b], writes=[kvb[dr]])
                    else:
                        op("dve", lambda e, c0=c0, dr=dr, Kp=Kp: e.tensor_copy(out=kv[dr][:, :, c0:c0 + 4].rearrange("p e c -> p c e"),
                                                                               in_=Kp[:].rearrange("p (c e) -> p c e", e=128)),
                           reads=[Kpb, boundb, bnd16b], writes=[kvb[dr]])

            run_pipeline(list(range(NT)), [b0, b1, b2, b3, b4, b5, b6, b7])
            if h + 1 < 4:
                xpre = load_head(h + 1)
            scan_all(0, fold=False)
            scan_all(1, fold=True)
            obk = {}

            def d0(c):
                T = c.T
                c.tc = slice(T * 128, (T + 1) * 128)
                c.S, c.Sb = g.bank_from("P")
                for dr in range(2):
                    mm_group(g, c.S[:, dr * 128:(dr + 1) * 128], c.Sb, [(big[("kt", dr)][:, c.tc], big[("qt", dr)][:, c.tc])],
                             reads=[bigb[("kt", dr)][T], bigb[("qt", dr)][T]])

            def d1(c):
                c.scm, c.scmb = r_scm.get()
                op("dve", lambda e: e.tensor_tensor(out=c.scm[:].rearrange("p (a t) -> p a t", a=2),
                                                    in0=c.S[:, 0:256].rearrange("p (a t) -> p a t", a=2),
                                                    in1=cst[:, 0:520].rearrange("p (a t) -> p a t", a=2)[:, :, 128:256],
                                                    op=ALU.mult), reads=[c.Sb, cstb], writes=[c.scmb])

            def d2(c):
                T = c.T
                B, tt = T // 4, T % 4
                if tt == 0:
                    obk[B] = g.bank_from("E")
                O, Ob = obk[B]
                oc = O[:, tt * 128:(tt + 1) * 128]
                rd = [Vb[T], c.scmb, kvb[0], kvb[1], bnd16b, bigb[("Qh", 0)][T], bigb[("Qh", 1)][T]]
                seq = [(V[:, T, :], c.scm[:, 0:128], oc, True), (V[:, T, :], c.scm[:, 128:256], oc, False)]
                for cq in range(4):
                    j = 4 * T + cq
                    occ = O[:, tt * 128 + cq * 32:tt * 128 + cq * 32 + 32]
                    qs = slice(T * 128 + cq * 32, T * 128 + cq * 32 + 32)
                    ib = 63 - j
                    sb_ap = kv[1][:, :, ib - 1] if ib >= 1 else bnd16[:]
                    seq.append((sb_ap, big[("Qh", 1)][:, qs], occ, False))
                    if j >= 1:
                        seq.append((kv[0][:, :, j - 1], big[("Qh", 0)][:, qs], occ, False))
                n = len(seq)
                for i, (l, r, o_ap, st) in enumerate(seq):
                    op("pe", lambda e, l=l, r=r, o_ap=o_ap, i=i, st=st: e.matmul(o_ap, lhsT=l, rhs=r, start=st, stop=(i == n - 1),
                                                                                 skip_group_check=True),
                       reads=rd, writes=[Ob], signal=(i == n - 1))
                if tt == 3:
                    osq, osqb = r_f512.get()
                    op("act", lambda e: e.activation(out=osq[:], in_=O[:], func=AF.Square), reads=[Ob], writes=[osqb])
                    Q, Qb = g.bank_from("K")
                    mm_group(g, Q[:], Qb, [(ones, osq[:])], reads=[cstb, osqb])
                    rt, rtb = r_f512.get()
                    op("act", lambda e: e.activation(out=rt[:], in_=Q[:], func=AF.Sqrt, scale=1.0 / 128.0, bias=RMS_EPS), reads=[Qb], writes=[rtb])
                    op("dve", lambda e: e.reciprocal(out=rt[:], in_=rt[:]), reads=[rtb], writes=[rtb])
                    t1, t1b = r_f512.get()
                    op("dve", lambda e: e.scalar_tensor_tensor(out=t1[:], in0=O[:], scalar=nrm[:, h:h + 1], in1=rt[:], op0=ALU.mult, op1=ALU.mult),
                       reads=[Ob, nrmb, rtb], writes=[t1b])
                    op("pool", lambda e: e.tensor_tensor(out=ybT[:, h, B * 512:(B + 1) * 512], in0=t1[:], in1=sgl[:, B * 512:(B + 1) * 512],
                                                         op=ALU.mult), reads=[t1b, sglb[B]], writes=[ybb[h][B]])

            run_pipeline(list(range(NT)), [d0, d1, d2])
        if stage == "mixer_dbg":
            g.dbg_evs2 = []
            kb.barrier()
            for nm, t_ in [("qt0", big[("qt", 0)]), ("Qh0", big[("Qh", 0)]), ("kt0", big[("kt", 0)]), ("qt1", big[("qt", 1)])]:
                dd = nc.dram_tensor("dbg_" + nm, [128, HALF], BF16, kind="ExternalOutput").ap()
                g.dbg_evs2.append(kb.dma("sp", dd[:, :], t_[:]))
            for nm, t_ in [("kv0", kv[0]), ("kv1", kv[1])]:
                dd = nc.dram_tensor("dbg_" + nm, [128, 128, 64], BF16, kind="ExternalOutput").ap()
                g.dbg_evs2.append(kb.dma("sp", dd[:, :, :], t_[:]))
            dd = nc.dram_tensor("dbg_dec0", [128, 64], F32, kind="ExternalOutput").ap()
            g.dbg_evs2.append(kb.dma("sp", dd[:, :], dec[0][:]))
            dd = nc.dram_tensor("dbg_V", [128, NT, 128], BF16, kind="ExternalOutput").ap()
            g.dbg_evs2.append(kb.dma("sp", dd[:, :, :], V[:]))
        kb.barrier()

    with ExitStack() as es:
        al = allocator(nc, es)
        cvw = al("cvw", [128, 4, 3], F32)
        cvwb = Buf()
        kb.dma("sp", cvw[:], g.convw.rearrange("(h p) c -> p h c", p=128), writes=[cvwb])
        w_cv = Ring(al, "w_cv", [128, 8, 384], BF16, 2)
        if stage not in ("mixer", "mixer_dbg"):
            zt = al("zt", [128, 8 * D], BF16)
            ztb = Buf()
            op("pool", lambda e: e.memset(zt[:], 0.0), writes=[ztb])
            xgv = g.Xg.rearrange("(n p j) d -> n p (j d)", p=128, j=8)
            for n in range(NE * CAP // 1024):
                kb.dma("sp", xgv[n], zt[:], reads=[ztb], writes=[g.xgb])
        zr = Ring(al, "z", [128, HALF + 2], F32, 2)
        cbsr = Ring(al, "cbs", [128, HALF], F32, 2)
        cvar = Ring(al, "cva", [128, HALF], F32, 2)
        r_f512 = Ring(al, "c_f512", [128, 512], F32, 3)
        for i in range(2):
            op("pool", lambda e, i=i: e.memset(zr.t[i][:, 0:1], 0.0), writes=[zr.b[i]])
        for cb in range(4):
            wt, wtb = w_cv.get()
            z, zb = zr.get()
            cbs, cbsb = cbsr.get()
            cva, cvab = cvar.get()
            for i, c0 in enumerate([C_B, C_C, C_H]):
                w_dma(g, wt[:, :, i * 128:(i + 1) * 128], g.w_in[:, c0 + cb * 128:c0 + (cb + 1) * 128], wtb)
            for B in range(5):
                cols = slice(B * 512, (B + 1) * 512) if B < 4 else slice(HALF, HALF + 8)
                ncol = 512 if B < 4 else 8
                ps = []
                for i in range(3):
                    if B == 4 and i == 0:
                        ps.append(None)
                        continue
                    Pc, Pcb = g.bank()
                    mm_group(g, Pc[:, 0:ncol], Pcb, [(wt[:, k, i * 128:(i + 1) * 128], xTo[:, k, cols]) for k in range(8)],
                             reads=[wtb, xTob[B]])
                    ps.append((Pc, Pcb))
                tmp, tmpb = r_f512.get()
                op("act", lambda e: e.activation(out=tmp[:, 0:ncol], in_=ps[1][0][:, 0:ncol], func=AF.Copy), reads=[ps[1][1]], writes=[tmpb])
                if B < 4:
                    op("dve", lambda e: e.tensor_tensor(out=z[:, 1 + B * 512:1 + (B + 1) * 512], in0=tmp[:], in1=ps[2][0][:], op=ALU.mult),
                       reads=[tmpb, ps[2][1]], writes=[zb])
                    op("act", lambda e: e.activation(out=cbs[:, cols], in_=ps[0][0][:], func=AF.Copy), reads=[ps[0][1]], writes=[cbsb])
                else:
                    op("dve", lambda e: e.tensor_tensor(out=z[:, HALF + 1:HALF + 2], in0=tmp[:, 0:1], in1=ps[2][0][:, 0:1], op=ALU.mult),
                       reads=[tmpb, ps[2][1]], writes=[zb])
            op("dve", lambda e: e.tensor_scalar(out=cva[:], in0=z[:, 0:HALF], scalar1=cvw[:, cb, 0:1], scalar2=None, op0=ALU.mult),
               reads=[zb, cvwb], writes=[cvab])
            op("dve", lambda e: e.scalar_tensor_tensor(out=cva[:], in0=z[:, 1:HALF + 1], scalar=cvw[:, cb, 1:2], in1=cva[:],
                                                       op0=ALU.mult, op1=ALU.add), reads=[zb, cvwb, cvab], writes=[cvab])
            op("dve", lambda e: e.scalar_tensor_tensor(out=cva[:], in0=z[:, 2:HALF + 2], scalar=cvw[:, cb, 2:3], in1=cva[:],
                                                       op0=ALU.mult, op1=ALU.add), reads=[zb, cvwb, cvab], writes=[cvab])
            op("pool", lambda e: e.tensor_tensor(out=yaT[:, cb, :], in0=cva[:], in1=cbs[:], op=ALU.mult),
               reads=[cvab, cbsb], writes=[yab[cb]])
        kb.barrier()

    if stage == "mixer_dbg":
        dya = nc.dram_tensor("dbg_ya", [128, 4, HALF], BF16, kind="ExternalOutput").ap()
        dyb = nc.dram_tensor("dbg_yb", [128, 4, HALF], BF16, kind="ExternalOutput").ap()
        g.dbg_evs = [kb.dma("sp", dya[:, :, :], yaT[:], reads=yab),
                     kb.dma("sp", dyb[:, :, :], ybT[:], reads=[ybb[hh][B] for hh in range(4) for B in range(4)])]
    with ExitStack() as es:
        al = allocator(nc, es)
        pa = al("pa", [128, 4, D], BF16)
        pb_ = al("pb", [128, 4, D], BF16)
        wo = al("wo", [128, 8, D], BF16)
        wga = al("wga", [128, 8, D], BF16)
        wgb = al("wgb", [128, 8, D], BF16)
        pab, pbb, wob, wgab, wgbb = Buf(), Buf(), Buf(), Buf(), Buf()
        w_dma(g, pa[:], g.p_a, pab)
        w_dma(g, pb_[:], g.p_b, pbb)
        w_dma(g, wo[:], g.w_o, wob)
        w_dma(g, wga[:], g.w_in[:, C_GA:C_GA + D], wgab)
        w_dma(g, wgb[:], g.w_in[:, C_GB:C_GB + D], wgbb)
        lng = al("lng", [128, D], F32)
        lnb = al("lnb", [128, D], F32)
        lngb, lnbb = Buf(), Buf()
        kb.dma("sp", lng[:], g.ln1g[0:1, :].broadcast_to([128, D]), writes=[lngb])
        kb.dma("sp", lnb[:], g.ln1b[0:1, :].broadcast_to([128, D]), writes=[lnbb])
        mixT = Ring(al, "mixT", [128, 8, 512], BF16, 2)
        r_x = Ring(al, "r_x", [128, D], F32, 3)
        r_r = Ring(al, "r_r", [128, D], F32, 4)
        r_st = Ring(al, "r_st", [128, 8], F32, 4)
        r_f512 = Ring(al, "g_f512", [128, 512], F32, 4)
        g.h_evs = []
        allyb = [ybb[hh][B] for hh in range(4) for B in range(4)]
        hdst = g.out if stage.startswith("mixer") else g.hs
        for B in range(4):
            bc = slice(B * 512, (B + 1) * 512)
            mt, mtb = mixT.get()
            for mc in range(8):
                ms = slice(mc * 128, (mc + 1) * 128)
                A_, Ab = g.bank()
                mm_group(g, A_[:], Ab, [(pa[:, k, ms], yaT[:, k, bc]) for k in range(4)], reads=[pab] + yab)
                B_, Bb = g.bank()
                mm_group(g, B_[:], Bb, [(pb_[:, k, ms], ybT[:, k, bc]) for k in range(4)], reads=[pbb] + allyb)
                GA, GAb = g.bank()
                mm_group(g, GA[:], GAb, [(wga[:, k, ms], xTo[:, k, bc]) for k in range(8)], reads=[wgab, xTob[B]])
                GB, GBb = g.bank()
                mm_group(g, GB[:], GBb, [(wgb[:, k, ms], xTo[:, k, bc]) for k in range(8)], reads=[wgbb, xTob[B]])
                sa, sab = r_f512.get()
                op("act", lambda e: e.activation(out=sa[:], in_=GA[:], func=AF.Sigmoid), reads=[GAb], writes=[sab])
                sb2, sbb = r_f512.get()
                op("act", lambda e: e.activation(out=sb2[:], in_=GB[:], func=AF.Sigmoid), reads=[GBb], writes=[sbb])
                op("dve", lambda e: e.tensor_tensor(out=sa[:], in0=sa[:], in1=A_[:], op=ALU.mult), reads=[sab, Ab], writes=[sab])
                op("dve", lambda e: e.tensor_tensor(out=sb2[:], in0=sb2[:], in1=B_[:], op=ALU.mult), reads=[sbb, Bb], writes=[sbb])
                op("pool", lambda e: e.tensor_tensor(out=mt[:, mc, :], in0=sa[:], in1=sb2[:], op=ALU.add), reads=[sab, sbb], writes=[mtb])
            for tt in range(4):
                T = 4 * B + tt
                xt_, xtb = r_x.get()
                kb.dma("sp", xt_[:], g.xo[T * 128:(T + 1) * 128, :], writes=[xtb])
                r_, rb = r_r.get()
                for hf in range(2):
                    O, Ob = g.bank()
                    mm_group(g, O[:], Ob, [(mt[:, k, tt * 128:(tt + 1) * 128], wo[:, k, hf * 512:(hf + 1) * 512]) for k in range(8)],
                             reads=[mtb, wob])
                    op("dve", lambda e: e.scalar_tensor_tensor(out=r_[:, hf * 512:(hf + 1) * 512], in0=xt_[:, hf * 512:(hf + 1) * 512],
                                                               scalar=ALPHA, in1=O[:], op0=ALU.mult, op1=ALU.add),
                       reads=[xtb, Ob], writes=[rb])
                layer_norm(g, r_, rb, xt_, xtb, lng, lngb, lnb, lnbb, r_st)
                ev = kb.dma("sp", hdst[T * 128:(T + 1) * 128, :], xt_[:], reads=[xtb])
                g.h_evs.append(ev)
        kb.barrier()
    g.out_evs = list(g.h_evs)


def layer_norm(g, r_, rb, o_, ob, lng, lngb, lnb, lnbb, r_st, last="pool", gmul="dve"):
    kb = g.kb
    op = kb.op
    st, stb = r_st.get()
    op("act", lambda e: e.activation(out=o_[:], in_=r_[:], func=AF.Identity, accum_out=st[:, 0:1]), reads=[rb], writes=[ob, stb])
    op("dve", lambda e: e.tensor_scalar(out=st[:, 1:2], in0=st[:, 0:1], scalar1=-1.0 / D, scalar2=None, op0=ALU.mult),
       reads=[stb], writes=[stb])
    op("act", lambda e: e.activation(out=o_[:], in_=r_[:], func=AF.Square, bias=st[:, 1:2], accum_out=st[:, 2:3]),
       reads=[rb, stb], writes=[ob, stb])
    op("act", lambda e: e.activation(out=st[:, 3:4], in_=st[:, 2:3], func=AF.Sqrt, scale=1.0 / D, bias=LN_EPS), reads=[stb], writes=[stb])
    op("dve", lambda e: e.reciprocal(out=st[:, 4:5], in_=st[:, 3:4]), reads=[stb], writes=[stb])
    op("dve", lambda e: e.tensor_scalar(out=r_[:], in0=r_[:], scalar1=st[:, 1:2], scalar2=st[:, 4:5], op0=ALU.add, op1=ALU.mult),
       reads=[rb, stb], writes=[rb])
    op(gmul, lambda e: e.tensor_tensor(out=r_[:], in0=r_[:], in1=lng[:], op=ALU.mult), reads=[rb, lngb], writes=[rb])
    return op(last, lambda e: e.tensor_tensor(out=o_[:], in0=r_[:], in1=lnb[:], op=ALU.add), reads=[rb, lnbb], writes=[ob])


def moe(g, stage="full"):
    if DBG.get("dense", 0):
        return moe_dense(g, stage)
    ne_run = ne_decl(stage)
    nc, kb = g.nc, g.kb
    cst, cstb = g.cst, g.cstb
    op = kb.op
    ident = cst[:, K_ID:K_ID + 128]
    I32 = mybir.dt.int32
    NR = NE * CAP
    Xg, xgb = g.Xg, g.xgb
    Yg = nc.dram_tensor("yg_scratch", [NR, D], BF16, kind="Internal").ap()
    ygb = Buf()
    with ExitStack() as es:
        al = allocator(nc, es)
        didx = al("didx", [128, NT, 4], I32)
        didxb = [Buf() for _ in range(NT)]
        gk = al("gk", [128, NT, 4], F32)
        gkb = [Buf() for _ in range(NT)]
        onesb = al("onesb", [128, 128], BF16)
        ltb = al("ltb", [128, 128], BF16)
        cb16 = Buf()
        op("dve", lambda e: e.tensor_copy(out=onesb[:], in_=cst[:, K_ONE:K_ONE + 128]), reads=[cstb], writes=[cb16])
        op("dve", lambda e: e.tensor_copy(out=ltb[:], in_=cst[:, K_LT:K_LT + 128]), reads=[cstb], writes=[cb16])

        wch = Ring(al, "wch", [128, 8, 512], BF16, 12)
        r_b1 = Ring(al, "r_b1", [128, 16], F32, 3)
        r_b2 = Ring(al, "r_b2", [1, D], BF16, 3)

        def prep_w(ex):
            st = Ctx()
            ch = []
            for q in [0, 2, 1, 3]:
                t_, b_ = wch.get()
                w_dma(g, t_[:], g.w1[ex, :, q * 512:(q + 1) * 512], b_)
                ch.append((q, t_, b_))
            st.w1c = {q: (t_, b_) for q, t_, b_ in ch}
            st.w2c = []
            for half in range(2):
                t_, b_ = wch.get()
                w_dma(g, t_[:], g.w2[ex, :, half * 512:(half + 1) * 512], b_)
                st.w2c.append((t_, b_))
            st.b1, st.b1b = r_b1.get()
            kb.dma("sp", st.b1[:], g.b1[ex, :, :], writes=[st.b1b])
            st.b2, st.b2b = r_b2.get()
            kb.dma("pool", st.b2[:], g.b2[ex:ex + 1, :], writes=[st.b2b])
            return st

        pw0 = prep_w(0) if ne_run > 0 else None

        with ExitStack() as es2:
            al2 = allocator(nc, es2)
            if xgb.w is None:
                zt = al2("zt", [128, 8 * D], BF16)
                ztb = Buf()
                op("pool", lambda e: e.memset(zt[:], 0.0), writes=[ztb])
                xgv = Xg.rearrange("(n p j) d -> n p (j d)", p=128, j=8)
                for n in range(NR // 1024):
                    kb.dma("sp", xgv[n], zt[:], reads=[ztb], writes=[xgb])
            wr = al2("wr", [128, 8, NE], F32)
            wrb = Buf()
            kb.dma("sp", wr[:], g.w_r.rearrange("(k p) n -> p k n", p=128), writes=[wrb])
            br = al2("br", [1, NE], F32)
            brb = Buf()
            kb.dma("sp", br[:], g.b_r[0:1, :], writes=[brb])
            msum = al2("msum", [128, NE], BF16)
            msumb = Buf()
            op("pool", lambda e: e.memset(msum[:], 0.0), writes=[msumb])
            r_h = Ring(al2, "r_h", [128, D], F32, 2)
            r_hb = Ring(al2, "r_hb", [128, D], BF16, 7)
            r_h32 = Ring(al2, "r_h32", [128, 8, 128], F32, 2)
            r_lg = Ring(al2, "r_lg", [128, 4, NE], F32, 3)
            r_mk = Ring(al2, "r_mk", [128, NE], BF16, 2)
            r_sm = Ring(al2, "r_sm", [128, 24], F32, 3)
            def r0(c):
                T = c.T
                c.ht, c.htb = r_h.get()
                kb.dma("sp", c.ht[:], g.hs[T * 128:(T + 1) * 128, :], writes=[c.htb])
                c.hb, c.hbb = r_hb.get()
                op("act", lambda e: e.activation(out=c.hb[:], in_=c.ht[:], func=AF.Copy), reads=[c.htb], writes=[c.hbb])
                c.Pt = []
                for half in range(2):
                    Pt, Ptb = g.bank_from("E")
                    for kk in range(4):
                        k = half * 4 + kk
                        op("pe", lambda e, k=k, kk=kk, Pt=Pt: e.transpose(out=Pt[:, kk * 128:(kk + 1) * 128], in_=c.ht[:, k * 128:(k + 1) * 128],
                                                                          identity=ident), reads=[c.htb, cstb], writes=[Ptb], signal=(kk == 3))
                    c.Pt.append((Pt, Ptb))

            def r1(c):
                c.h32, c.h32b = r_h32.get()
                Pt, Ptb = c.Pt[0]
                op("act", lambda e: e.activation(out=c.h32[:, 0:4, :], in_=Pt[:].rearrange("p (k t) -> p k t", k=4), func=AF.Copy),
                   reads=[Ptb], writes=[c.h32b])
                Pt, Ptb = c.Pt[1]
                op("dve", lambda e: e.tensor_copy(out=c.h32[:, 4:8, :], in_=Pt[:].rearrange("p (k t) -> p k t", k=4)),
                   reads=[Ptb], writes=[c.h32b])

            def r2(c):
                c.L, c.Lb = g.bank_from("P")
                pairs = [(c.h32[:, k, :], wr[:, k, :]) for k in range(8)] + [(cst[0:1, K_ONE:K_ONE + 128], br[0:1, :])]
                mm_group(g, c.L[:, 0:NE], c.Lb, pairs, reads=[c.h32b, wrb, brb, cstb])

            def r3(c):
                c.lg, c.lgb = r_lg.get()
                c.sm, c.smb = r_sm.get()
                c.mk_, c.mkb = r_mk.get()
                lg, lgb, sm, smb, mk_, mkb = c.lg, c.lgb, c.sm, c.smb, c.mk_, c.mkb
                op("dve", lambda e: e.tensor_copy(out=lg[:, 0, :], in_=c.L[:, 0:NE]), reads=[c.Lb], writes=[lgb])
                op("dve", lambda e: e.max(out=sm[:, 0:8], in_=lg[:, 0, :]), reads=[lgb], writes=[smb])
                op("dve", lambda e: e.tensor_scalar(out=mk_[:], in0=lg[:, 0, :], scalar1=sm[:, 3:4], scalar2=None, op0=ALU.is_ge),
                   reads=[lgb, smb], writes=[mkb])

            def r4(c):
                c.Pp, c.Ppb = g.bank_from("K")
                mm_group(g, c.Pp[:, 0:NE], c.Ppb, [(ltb[:], c.mk_[:]), (onesb[:], msum[:])], reads=[cb16, c.mkb, msumb])
                op("pool", lambda e: e.tensor_tensor(out=msum[:], in0=msum[:], in1=c.mk_[:], op=ALU.add), reads=[c.mkb, msumb], writes=[msumb])

            def r5(c):
                T = c.T
                lg, lgb, sm, smb = c.lg, c.lgb, c.sm, c.smb
                Pp, Ppb = c.Pp, c.Ppb
                op("dve", lambda e: e.tensor_tensor(out=lg[:, 1, :], in0=Pp[:, 0:NE], in1=cst[:, K_EC:K_EC + NE], op=ALU.add),
                   reads=[Ppb, cstb], writes=[lgb])
                op("dve", lambda e: e.tensor_scalar(out=lg[:, 2, :], in0=Pp[:, 0:NE], scalar1=float(CAP), scalar2=1.0e7, op0=ALU.is_ge, op1=ALU.mult),
                   reads=[Ppb], writes=[lgb])
                op("dve", lambda e: e.tensor_tensor(out=lg[:, 1, :], in0=lg[:, 1, :], in1=lg[:, 2, :], op=ALU.add), reads=[lgb], writes=[lgb])
                for k in range(4):
                    op("dve", lambda e, k=k: e.scalar_tensor_tensor(out=lg[:, 3, :], in0=lg[:, 0, :], scalar=sm[:, k:k + 1], in1=lg[:, 1, :],
                                                                    op0=ALU.is_equal, op1=ALU.mult, accum_out=sm[:, 8 + k:9 + k]),
                       reads=[lgb, smb], writes=[lgb, smb])
                op("dve", lambda e: e.tensor_copy(out=didx[:, T, :], in_=sm[:, 8:12]), reads=[smb], writes=[didxb[T]])
                op("dve", lambda e: e.tensor_scalar(out=sm[:, 12:13], in0=sm[:, 0:1], scalar1=-1.0, scalar2=None, op0=ALU.mult),
                   reads=[smb], writes=[smb])
                op("act", lambda e: e.activation(out=sm[:, 13:17], in_=sm[:, 0:4], func=AF.Exp, bias=sm[:, 12:13], accum_out=sm[:, 17:18]),
                   reads=[smb], writes=[smb])
                op("dve", lambda e: e.reciprocal(out=sm[:, 18:19], in_=sm[:, 17:18]), reads=[smb], writes=[smb])
                op("dve", lambda e: e.tensor_scalar(out=sm[:, 19:23], in0=sm[:, 8:12], scalar1=1.0e6, scalar2=None, op0=ALU.is_lt),
                   reads=[smb], writes=[smb])
                op("dve", lambda e: e.scalar_tensor_tensor(out=gk[:, T, :], in0=sm[:, 13:17], scalar=sm[:, 18:19], in1=sm[:, 19:23],
                                                           op0=ALU.mult, op1=ALU.mult), reads=[smb], writes=[gkb[T]])
                for k in range(4):
                    kb.idma(Xg[:, :], c.hb[:, :], didx[:, T, k:k + 1], True, NR - 1, reads=[c.hbb, didxb[T]], writes=[xgb])

            stages = [r0, r1, r2, r3, r4, r5]
            cs = []
            for T in range(NT):
                c = Ctx()
                c.T = T
                cs.append(c)
            for step in range(NT + len(stages) - 1):
                for si in range(len(stages)):
                    i = step - si
                    if 0 <= i < NT:
                        stages[si](cs[i])
            kb.barrier()

        with ExitStack() as es2:
            al2 = allocator(nc, es2)
            r_xg = Ring(al2, "r_xg", [128, CAP // 128, D], BF16, 2)
            r_xT = Ring(al2, "r_xT", [128, 8, CAP], BF16, 2)
            r_aT = Ring(al2, "r_aT", [128, 8, CAP], BF16, 2)
            r_hg = Ring(al2, "r_hg", [128, CAP], F32, 2)
            r_sg = Ring(al2, "r_sgm", [128, CAP], BF16, 2)
            r_hl = Ring(al2, "r_hl", [128, CAP], F32, 2)
            r_a1 = Ring(al2, "r_a1", [128, CAP], BF16, 2)
            r_y = Ring(al2, "r_y", [128, D], BF16, 3)
            NJ = CAP // 128
            def prep(ex, st=None):
                if st is None:
                    st = prep_w(ex)
                st.xg, st.xgtb = r_xg.get()
                kb.dma("sp", st.xg[:], Xg[ex * CAP:(ex + 1) * CAP, :].rearrange("(j p) d -> p j d", p=128), reads=[xgb], writes=[st.xgtb])
                return st

            def transposes(st):
                xg, xgtb = st.xg, st.xgtb
                st.xT, st.xTb = r_xT.get()
                xT, xTb = st.xT, st.xTb
                for kp in range(4):
                    Pt, Ptb = g.bank()
                    Pv = Pt[:].bitcast(BF16)
                    n_tr = 2 * NJ
                    i = 0
                    for kk in range(2):
                        for j in range(NJ):
                            k = 2 * kp + kk
                            i += 1
                            op("pe", lambda e, k=k, kk=kk, j=j: e.transpose(out=Pv[:, kk * CAP + j * 128:kk * CAP + (j + 1) * 128],
                                                                            in_=xg[:, j, k * 128:(k + 1) * 128], identity=g.idb[:]),
                               reads=[xgtb, g.idbb], writes=[Ptb], signal=(i == n_tr))
                    src = Pv[:, 0:2 * CAP].rearrange("p (k t) -> p k t", k=2)
                    if kp % 2 == 0:
                        op("act", lambda e: e.activation(out=xT[:, 2 * kp:2 * kp + 2, :], in_=src, func=AF.Copy), reads=[Ptb], writes=[xTb])
                    else:
                        op("dve", lambda e: e.tensor_copy(out=xT[:, 2 * kp:2 * kp + 2, :], in_=src), reads=[Ptb], writes=[xTb])

            cur = prep(0, pw0) if ne_run > 0 else None
            if cur is not None:
                transposes(cur)
            for ex in range(ne_run):
                st = cur
                nxt = prep(ex + 1) if ex + 1 < ne_run else None
                w1c, w2c, b1, b1b, b2, b2b, xT, xTb = st.w1c, st.w2c, st.b1, st.b1b, st.b2, st.b2b, st.xT, st.xTb
                aT, aTb = r_aT.get()
                for f in range(8):
                    fs = slice((f % 4) * 128, (f % 4 + 1) * 128)
                    wg, wgb_ = w1c[f // 4]
                    wl, wlb_ = w1c[2 + f // 4]
                    Hg, Hgb = g.bank()
                    mm_group(g, Hg[:, 0:CAP], Hgb, [(wg[:, k, fs], xT[:, k, :]) for k in range(8)], reads=[wgb_, xTb])
                    Hl, Hlb = g.bank()
                    mm_group(g, Hl[:, 0:CAP], Hlb, [(wl[:, k, fs], xT[:, k, :]) for k in range(8)], reads=[wlb_, xTb])
                    hg, hgb = r_hg.get()
                    op("dve", lambda e: e.tensor_scalar(out=hg[:], in0=Hg[:, 0:CAP], scalar1=b1[:, f:f + 1], scalar2=7.0, op0=ALU.add, op1=ALU.min),
                       reads=[Hgb, b1b], writes=[hgb])
                    sg, sgb = r_sg.get()
                    op("act", lambda e: e.activation(out=sg[:], in_=hg[:], func=AF.Sigmoid, scale=1.702), reads=[hgb], writes=[sgb])
                    hl, hlb = r_hl.get()
                    op("dve", lambda e: e.tensor_scalar(out=hl[:], in0=Hl[:, 0:CAP], scalar1=b1[:, 8 + f:9 + f], scalar2=7.0, op0=ALU.add, op1=ALU.min),
                       reads=[Hlb, b1b], writes=[hlb])
                    op("dve", lambda e: e.tensor_scalar(out=hl[:], in0=hl[:], scalar1=-7.0, scalar2=1.0, op0=ALU.max, op1=ALU.add),
                       reads=[hlb], writes=[hlb])
                    a1, a1b = r_a1.get()
                    op("pool", lambda e: e.tensor_tensor(out=a1[:], in0=hg[:], in1=sg[:], op=ALU.mult), reads=[hgb, sgb], writes=[a1b])
                    op("dve", lambda e: e.tensor_tensor(out=aT[:, f, :], in0=a1[:], in1=hl[:], op=ALU.mult), reads=[a1b, hlb], writes=[aTb])
                if nxt is not None:
                    transposes(nxt)
                for tt in range(NJ):
                    ysb, ysbb = r_y.get()
                    for half in range(2):
                        w2t, w2b = w2c[half]
                        Y, Yb = g.bank()
                        pairs = [(aT[:, f, tt * 128:(tt + 1) * 128], w2t[:, f, :]) for f in range(8)]
                        pairs.append((onesb[0:1, :], b2[0:1, half * 512:(half + 1) * 512]))
                        mm_group(g, Y[:], Yb, pairs, reads=[aTb, w2b, b2b, cb16])
                        if half == 0:
                            op("act", lambda e: e.activation(out=ysb[:, 0:512], in_=Y[:], func=AF.Copy), reads=[Yb], writes=[ysbb])
                        else:
                            op("dve", lambda e: e.tensor_copy(out=ysb[:, 512:1024], in_=Y[:]), reads=[Yb], writes=[ysbb])
                    r0 = ex * CAP + tt * 128
                    kb.dma("sp", Yg[r0:r0 + 128, :], ysb[:], reads=[ysbb], writes=[ygb])
                cur = nxt
            kb.barrier()

        with ExitStack() as es2:
            al2 = allocator(nc, es2)
            lng = al2("lng2", [128, D], F32)
            lnb = al2("lnb2", [128, D], F32)
            lngb, lnbb = Buf(), Buf()
            kb.dma("sp", lng[:], g.ln2g[0:1, :].broadcast_to([128, D]), writes=[lngb])
            kb.dma("sp", lnb[:], g.ln2b[0:1, :].broadcast_to([128, D]), writes=[lnbb])
            r_h = Ring(al2, "f_h", [128, D], F32, 2)
            r_st = Ring(al2, "f_st", [128, 8], F32, 2)
            r_yk = Ring(al2, "f_yk", [128, 4, D], BF16, 4)
            r_ac = Ring(al2, "f_ac", [128, D], F32, 2)
            for i in range(4):
                op("pool", lambda e, i=i: e.memset(r_yk.t[i][:], 0.0), writes=[r_yk.b[i]])
            g.out_evs = []

            def gathers(T):
                yk, ykb = r_yk.get()
                if ne_run == NE:
                    for k in range(4):
                        kb.idma(yk[:, k, :], Yg[:, :], didx[:, T, k:k + 1], False, NR - 1, reads=[didxb[T], ygb], writes=[ykb])
                return yk, ykb
            pend = [gathers(0), gathers(1), gathers(2)]
            for T in range(NT):
                ht, htb = r_h.get()
                kb.dma("sp", ht[:], g.hs[T * 128:(T + 1) * 128, :], writes=[htb])
                yk, ykb = pend.pop(0)
                if T + 3 < NT:
                    pend.append(gathers(T + 3))
                ac, acb = r_ac.get()
                op("dve", lambda e: e.tensor_scalar(out=ac[:], in0=yk[:, 0, :], scalar1=gk[:, T, 0:1], scalar2=None, op0=ALU.mult),
                   reads=[ykb, gkb[T]], writes=[acb])
                for k in range(1, 4):
                    op("dve", lambda e, k=k: e.scalar_tensor_tensor(out=ac[:], in0=yk[:, k, :], scalar=gk[:, T, k:k + 1], in1=ac[:],
                                                                    op0=ALU.mult, op1=ALU.add), reads=[ykb, gkb[T], acb], writes=[acb])
                op("dve", lambda e: e.scalar_tensor_tensor(out=ac[:], in0=ht[:], scalar=ALPHA, in1=ac[:], op0=ALU.mult, op1=ALU.add),
                   reads=[htb, acb], writes=[acb])
                layer_norm(g, ac, acb, ht, htb, lng, lngb, lnb, lnbb, r_st, last="dve")
                g.out_evs.append(kb.dma("sp", g.out[T * 128:(T + 1) * 128, :], ht[:], reads=[htb]))
            kb.barrier()


def moe_dense(g, stage="full"):
    ne_run = ne_decl(stage)
    nc, kb = g.nc, g.kb
    cst, cstb = g.cst, g.cstb
    op = kb.op
    ident = cst[:, K_ID:K_ID + 128]
    with ExitStack() as es:
        al = allocator(nc, es)
        hT = al("hT", [128, 8, HALF], BF16)
        hTb = [Buf() for _ in range(4)]
        gate = al("gate", [128, NT, NE], F32)
        gateb = [Buf() for _ in range(NT)]
        acc = al("acc", [128, NT, D], F32)
        accb = [Buf() for _ in range(NT)]
        onesb = al("onesb", [1, 128], BF16)
        onesbb = Buf()
        op("dve", lambda e: e.tensor_copy(out=onesb[:], in_=cst[0:1, K_ONE:K_ONE + 128]), reads=[cstb], writes=[onesbb])

        with ExitStack() as es2:
            al2 = allocator(nc, es2)
            wr = al2("wr", [128, 8, NE], F32)
            wrb = Buf()
            kb.dma("sp", wr[:], g.w_r.rearrange("(k p) n -> p k n", p=128), writes=[wrb])
            br = al2("br", [1, NE], F32)
            brb = Buf()
            kb.dma("sp", br[:], g.b_r[0:1, :], writes=[brb])
            r_h = Ring(al2, "r_h", [128, D], F32, 2)
            r_h32 = Ring(al2, "r_h32", [128, 8, 128], F32, 2)
            r_lg = Ring(al2, "r_lg", [128, 3, NE], F32, 2)
            r_sm = Ring(al2, "r_sm", [128, 16], F32, 2)
            for T in range(NT if DBG.get("router", 1) else 0):
                ht, htb = r_h.get()
                kb.dma("sp", ht[:], g.hs[T * 128:(T + 1) * 128, :], writes=[htb])
                h32, h32b = r_h32.get()
                for half in range(2):
                    Pt, Ptb = g.bank()
                    for kk in range(4 if DBG.get("tr", 1) else 0):
                        k = half * 4 + kk
                        op("pe", lambda e, k=k, kk=kk: e.transpose(out=Pt[:, kk * 128:(kk + 1) * 128], in_=ht[:, k * 128:(k + 1) * 128],
                                                                   identity=ident), reads=[htb, cstb], writes=[Ptb], signal=(kk == 3))
                    if DBG.get("r_act", 1):
                        op("act", lambda e: e.activation(out=h32[:, half * 4:(half + 1) * 4, :], in_=Pt[:].rearrange("p (k t) -> p k t", k=4),
                                                         func=AF.Copy), reads=[Ptb], writes=[h32b])
                    if DBG.get("r_dve", 1):
                      op("dve", lambda e: e.tensor_copy(out=hT[:, half * 4:(half + 1) * 4, T * 128:(T + 1) * 128],
                                                      in_=Pt[:].rearrange("p (k t) -> p k t", k=4)), reads=[Ptb], writes=[hTb[T // 4]])
                if not DBG.get("router_mm", 1):
                    if DBG.get("r_pool", 1):
                        op("pool", lambda e: e.memset(gate[:, T, :], 0.0), writes=[gateb[T]])
                    continue
                L, Lb = g.bank()
                pairs = [(h32[:, k, :], wr[:, k, :]) for k in range(8)] + [(cst[0:1, K_ONE:K_ONE + 128], br[0:1, :])]
                mm_group(g, L[:, 0:NE], Lb, pairs, reads=[h32b, wrb, brb, cstb])
                lg, lgb = r_lg.get()
                sm, smb = r_sm.get()
                op("dve", lambda e: e.tensor_copy(out=lg[:, 0, :], in_=L[:, 0:NE]), reads=[Lb], writes=[lgb])
                op("dve", lambda e: e.max(out=sm[:, 0:8], in_=lg[:, 0, :]), reads=[lgb], writes=[smb])
                op("dve", lambda e: e.tensor_scalar(out=lg[:, 1, :], in0=lg[:, 0, :], scalar1=sm[:, 3:4], scalar2=None, op0=ALU.is_ge),
                   reads=[lgb, smb], writes=[lgb])
                op("dve", lambda e: e.tensor_scalar(out=sm[:, 8:9], in0=sm[:, 0:1], scalar1=-1.0, scalar2=None, op0=ALU.mult),
                   reads=[smb], writes=[smb])
                op("act", lambda e: e.activation(out=lg[:, 2, :], in_=lg[:, 0, :], func=AF.Exp, bias=sm[:, 8:9]), reads=[lgb, smb], writes=[lgb])
                op("dve", lambda e: e.scalar_tensor_tensor(out=lg[:, 2, :], in0=lg[:, 2, :], scalar=1.0, in1=lg[:, 1, :], op0=ALU.mult,
                                                           op1=ALU.mult, accum_out=sm[:, 9:10]), reads=[lgb], writes=[lgb, smb])
                op("dve", lambda e: e.reciprocal(out=sm[:, 10:11], in_=sm[:, 9:10]), reads=[smb], writes=[smb])
                op("dve", lambda e: e.tensor_scalar(out=gate[:, T, :], in0=lg[:, 2, :], scalar1=sm[:, 10:11], scalar2=None, op0=ALU.mult),
                   reads=[lgb, smb], writes=[gateb[T]])
            kb.barrier()

        with ExitStack() as es2:
            al2 = allocator(nc, es2)
            wch = Ring(al2, "wch", [128, 8, 512], BF16, 7)
            r_b1 = Ring(al2, "r_b1", [128, 16], F32, 2)
            r_b2 = Ring(al2, "r_b2", [1, D], BF16, 2)
            r_aT = Ring(al2, "r_aT", [128, 8, 512], BF16, 2)
            r_hg = Ring(al2, "r_hg", [128, 512], F32, 2)
            r_sg = Ring(al2, "r_sgm", [128, 512], BF16, 2)
            r_hl = Ring(al2, "r_hl", [128, 512], F32, 2)
            r_a1 = Ring(al2, "r_a1", [128, 512], BF16, 2)
            if ne_run == 0:
                for T in range(NT):
                    op("pool", lambda e: e.memset(acc[:, T, :], 0.0), writes=[accb[T]])
            for ex in range(ne_run):
                ch = []
                for q in [0, 2, 1, 3]:
                    t_, b_ = wch.get()
                    w_dma(g, t_[:], g.w1[ex, :, q * 512:(q + 1) * 512], b_)
                    ch.append((q, t_, b_))
                w1c = {q: (t_, b_) for q, t_, b_ in ch}
                w2c = []
                for half in range(2):
                    t_, b_ = wch.get()
                    w_dma(g, t_[:], g.w2[ex, :, half * 512:(half + 1) * 512], b_)
                    w2c.append((t_, b_))
                b1, b1b = r_b1.get()
                kb.dma("sp", b1[:], g.b1[ex, :, :], writes=[b1b])
                b2, b2b = r_b2.get()
                kb.dma("pool", b2[:], g.b2[ex:ex + 1, :], writes=[b2b])
                for B in range(4):
                    bc = slice(B * 512, (B + 1) * 512)
                    aT, aTb = r_aT.get()
                    for f in range(8):
                        fs = slice((f % 4) * 128, (f % 4 + 1) * 128)
                        wg, wgb_ = w1c[f // 4]
                        wl, wlb_ = w1c[2 + f // 4]
                        Hg, Hgb = g.bank()
                        mm_group(g, Hg[:], Hgb, [(wg[:, k, fs], hT[:, k, bc]) for k in range(8)], reads=[wgb_, hTb[B]])
                        Hl, Hlb = g.bank()
                        mm_group(g, Hl[:], Hlb, [(wl[:, k, fs], hT[:, k, bc]) for k in range(8)], reads=[wlb_, hTb[B]])
                        hg, hgb = r_hg.get()
                        op("dve", lambda e: e.tensor_scalar(out=hg[:], in0=Hg[:], scalar1=b1[:, f:f + 1], scalar2=7.0, op0=ALU.add, op1=ALU.min),
                           reads=[Hgb, b1b], writes=[hgb])
                        sg, sgb = r_sg.get()
                        op("act", lambda e: e.activation(out=sg[:], in_=hg[:], func=AF.Sigmoid, scale=1.702), reads=[hgb], writes=[sgb])
                        hl, hlb = r_hl.get()
                        op("dve", lambda e: e.tensor_scalar(out=hl[:], in0=Hl[:], scalar1=b1[:, 8 + f:9 + f], scalar2=7.0, op0=ALU.add, op1=ALU.min),
                           reads=[Hlb, b1b], writes=[hlb])
                        op("dve", lambda e: e.tensor_scalar(out=hl[:], in0=hl[:], scalar1=-7.0, scalar2=1.0, op0=ALU.max, op1=ALU.add),
                           reads=[hlb], writes=[hlb])
                        a1, a1b = r_a1.get()
                        op("pool", lambda e: e.tensor_tensor(out=a1[:], in0=hg[:], in1=sg[:], op=ALU.mult), reads=[hgb, sgb], writes=[a1b])
                        op("dve", lambda e: e.tensor_tensor(out=aT[:, f, :], in0=a1[:], in1=hl[:], op=ALU.mult), reads=[a1b, hlb], writes=[aTb])
                    for tt in range(4):
                        T = 4 * B + tt
                        for half in range(2):
                            w2t, w2b = w2c[half]
                            Y, Yb = g.bank()
                            pairs = [(aT[:, f, tt * 128:(tt + 1) * 128], w2t[:, f, :]) for f in range(8)]
                            pairs.append((onesb[0:1, :], b2[0:1, half * 512:(half + 1) * 512]))
                            mm_group(g, Y[:], Yb, pairs, reads=[aTb, w2b, b2b, onesbb])
                            dst = acc[:, T, half * 512:(half + 1) * 512]
                            if ex == 0:
                                op("dve", lambda e: e.tensor_scalar(out=dst, in0=Y[:], scalar1=gate[:, T, ex:ex + 1], scalar2=None, op0=ALU.mult),
                                   reads=[Yb, gateb[T]], writes=[accb[T]])
                            else:
                                op("dve", lambda e: e.scalar_tensor_tensor(out=dst, in0=Y[:], scalar=gate[:, T, ex:ex + 1], in1=dst,
                                                                           op0=ALU.mult, op1=ALU.add), reads=[Yb, gateb[T], accb[T]], writes=[accb[T]])
            kb.barrier()

        with ExitStack() as es2:
            al2 = allocator(nc, es2)
            lng = al2("lng2", [128, D], F32)
            lnb = al2("lnb2", [128, D], F32)
            lngb, lnbb = Buf(), Buf()
            kb.dma("sp", lng[:], g.ln2g[0:1, :].broadcast_to([128, D]), writes=[lngb])
            kb.dma("sp", lnb[:], g.ln2b[0:1, :].broadcast_to([128, D]), writes=[lnbb])
            r_h = Ring(al2, "f_h", [128, D], F32, 2)
            r_st = Ring(al2, "f_st", [128, 8], F32, 2)
            g.out_evs = []
            for T in range(NT):
                ht, htb = r_h.get()
                kb.dma("sp", ht[:], g.hs[T * 128:(T + 1) * 128, :], writes=[htb])
                op("dve", lambda e: e.scalar_tensor_tensor(out=acc[:, T, :], in0=ht[:], scalar=ALPHA, in1=acc[:, T, :], op0=ALU.mult, op1=ALU.add),
                   reads=[htb, accb[T]], writes=[accb[T]])
                layer_norm(g, acc[:, T, :], accb[T], ht, htb, lng, lngb, lnb, lnbb, r_st)
                g.out_evs.append(kb.dma("sp", g.out[T * 128:(T + 1) * 128, :], ht[:], reads=[htb]))
            kb.barrier()


def shard_inputs(inputs, stage):
    x = np.asarray(inputs["x"], np.float32)
    w_in = np.asarray(inputs["w_in"], np.float32)[0]
    w_in_sw = w_in.copy()
    w_in_sw[:, C_FF:C_FF + 512] = w_in[:, C_FB:C_FB + 512]
    w_in_sw[:, C_FB:C_FB + 512] = w_in[:, C_FF:C_FF + 512]
    conv_w = np.asarray(inputs["conv_w"], np.float32)[0]
    lbl = np.asarray(inputs["hgrn_lb_logits"], np.float32)
    consts = make_consts()
    maps = []
    for c in range(8):
        b, hf = c // 2, c % 2
        xl = x[b] if hf == 0 else x[b][::-1]
        cw = conv_w if hf == 0 else conv_w[::-1]
        ll = lbl if hf == 0 else lbl[::-1]
        m = {
            "xT": np.ascontiguousarray(xl.T),
            "xo": np.ascontiguousarray(xl[:HALF]),
            "w_in": w_in if hf == 0 else w_in_sw,
            "convw_t": np.ascontiguousarray(cw.T),
            "lbl": np.ascontiguousarray(ll.reshape(1, 2048)),
            "lbl_t": np.ascontiguousarray(ll.reshape(4, 512).T),
            "normw_t": np.ascontiguousarray(np.asarray(inputs["hgrn_norm_w"], np.float32)[0].reshape(4, 128).T),
            "p_a": np.asarray(inputs["p_a"], np.float32)[0],
            "p_b": np.asarray(inputs["p_b"], np.float32)[0],
            "w_o": np.asarray(inputs["w_o"], np.float32)[0],
            "ln1_g": np.asarray(inputs["ln1_g"], np.float32),
            "ln1_b": np.asarray(inputs["ln1_b"], np.float32),
            "consts": consts,
        }
        if stage in FULLS:
            b1 = np.asarray(inputs["b1"], np.float32)[0]
            m.update({
                "w_router": np.asarray(inputs["w_router"], np.float32)[0],
                "b_router": np.asarray(inputs["b_router"], np.float32),
                "ln2_g": np.asarray(inputs["ln2_g"], np.float32),
                "ln2_b": np.asarray(inputs["ln2_b"], np.float32),
            })
            if "router" not in stage:
                nd = ne_decl(stage)
                m.update({
                    "w1": np.asarray(inputs["w1"], np.float32)[0][:nd],
                    "b1t": np.ascontiguousarray(b1.reshape(NE, 16, 128).transpose(0, 2, 1))[:nd],
                    "w2": np.asarray(inputs["w2"], np.float32)[0][:nd],
                    "b2": np.asarray(inputs["b2"], np.float32)[0][:nd],
                })
        if stage.startswith("m_"):
            hl = inputs["h_in"][b] if hf == 0 else inputs["h_in"][b][::-1]
            m["h_in"] = np.ascontiguousarray(hl[:HALF])
        maps.append(m)
    return maps


def run(inputs, stage="full", key="out"):
    nc = build_nc(stage)
    maps = shard_inputs(inputs, stage)
    res = run_bass_kernel_spmd(nc, maps, core_ids=list(range(8)))
    out = np.zeros((4, SEQ, D), np.float32)
    for c in range(8):
        b, hf = c // 2, c % 2
        y = np.asarray(res.results[c][key], np.float32)
        if hf == 0:
            out[b, :HALF] = y
        else:
            out[b, HALF:] = y[::-1]
    return out


def kernel(**inputs):
    return run(inputs, "full", "out")
```
